# Optimizing a Trainium2 kernel written in Bass

```python
import math
import jax, jax.numpy as jnp
from jax import lax
import numpy as np

D_MODEL = 1024
BATCH = 8
SEQ = 2048
DEPTH = 2

HEAD_DIM = 64
ATTN_WIDTH = D_MODEL // 2
ATTN_HEADS = ATTN_WIDTH // HEAD_DIM
KV_HEADS = 2
GROUP = ATTN_HEADS // KV_HEADS
IDX_HEADS = 16
IDX_DIM = 64
INDEX_TOPK = 256
Q_BLOCK = 128
CONV_WIDTH = D_MODEL // 4
CONV_KERNEL = 31
POOL_WIDTH = D_MODEL // 4
POOL_GROUPS = 4
POOL_GROUP_DIM = POOL_WIDTH // POOL_GROUPS
POOL_WINDOWS = (2, 4, 8, 16)
POOL_PAD = max(POOL_WINDOWS)
MIX_WIDTH = ATTN_WIDTH + CONV_WIDTH + POOL_WIDTH
D_FF = 4 * D_MODEL
PLE_DIM = 256
ROPE_THETA = 500000.0
ROPE_DIM = HEAD_DIM // 4
MAX_POS_OFFSET = 1024
NORM_EPS = 1e-6
LN_EPS = 1e-5

IN_SIZES = (
    ATTN_WIDTH,
    KV_HEADS * HEAD_DIM,
    KV_HEADS * HEAD_DIM,
    IDX_HEADS * IDX_DIM,
    IDX_DIM,
    IDX_HEADS,
    2 * CONV_WIDTH,
    POOL_WIDTH,
)
IN_WIDTH = int(sum(IN_SIZES))
IN_SPLITS = [int(v) for v in np.cumsum(IN_SIZES)[:-1]]

kernel_name = "hybrid_dsa_conformer_pool_block"


def rmsnorm(x, g):
    xf = x.astype(jnp.float32)
    y = xf * lax.rsqrt(jnp.mean(xf * xf, axis=-1, keepdims=True) + NORM_EPS)
    return (y * g.astype(jnp.float32)).astype(x.dtype)


def layernorm(x, g, b):
    xf = x.astype(jnp.float32)
    mu = jnp.mean(xf, axis=-1, keepdims=True)
    var = jnp.mean(jnp.square(xf - mu), axis=-1, keepdims=True)
    y = (xf - mu) * lax.rsqrt(var + LN_EPS)
    return (y * g.astype(jnp.float32) + b.astype(jnp.float32)).astype(x.dtype)


def rope_tables(positions):
    inv_freq = ROPE_THETA ** (-jnp.arange(0, ROPE_DIM, 2, dtype=jnp.float32) / ROPE_DIM)
    ang = positions.astype(jnp.float32)[..., None] * inv_freq
    return jnp.cos(ang)[:, :, None, :], jnp.sin(ang)[:, :, None, :]


def partial_rope(x, cos, sin):
    half = ROPE_DIM // 2
    cos = cos.astype(x.dtype)
    sin = sin.astype(x.dtype)
    x1 = x[..., :half]
    x2 = x[..., half:ROPE_DIM]
    return jnp.concatenate([x1 * cos - x2 * sin, x2 * cos + x1 * sin, x[..., ROPE_DIM:]], axis=-1)


def dsa_sparse_attention(q, k, v, qi, ki, wi):
    B, S = q.shape[0], q.shape[1]
    n_blk = S // Q_BLOCK
    k_sel = min(INDEX_TOPK, S // 4)
    idx_scale = (IDX_DIM ** -0.5) * (IDX_HEADS ** -0.5)
    att_scale = HEAD_DIM ** -0.5
    neg = jnp.finfo(jnp.float32).min
    key_pos = jnp.arange(S)
    b_idx = jnp.arange(B)[:, None, None]
    ki_f = ki.astype(jnp.float32)

    def to_blocks(t):
        return jnp.moveaxis(t.reshape((B, n_blk, Q_BLOCK) + t.shape[2:]), 1, 0)

    q_b = to_blocks(q.reshape(B, S, KV_HEADS, GROUP, HEAD_DIM))
    qi_b = to_blocks(qi)
    wi_b = to_blocks(wi)
    t_b = key_pos.reshape(n_blk, Q_BLOCK)

    def block(args):
        q_blk, qi_blk, wi_blk, t_blk = args
        dots = jnp.einsum('bthd,bsd->bths', qi_blk.astype(jnp.float32), ki_f)
        idx = jnp.einsum('bths,bth->bts', jax.nn.relu(dots), wi_blk.astype(jnp.float32)) * idx_scale
        causal = key_pos[None, :] <= t_blk[:, None]
        idx = jnp.where(causal[None], idx, -jnp.inf)
        _, sel = lax.top_k(idx, k_sel)
        valid = sel <= t_blk[None, :, None]
        k_g = k[b_idx, sel]
        v_g = v[b_idx, sel]
        s = jnp.einsum('bthgd,btnhd->bthgn', q_blk, k_g).astype(jnp.float32) * att_scale
        s = jnp.where(valid[:, :, None, None, :], s, neg)
        pr = jax.nn.softmax(s, axis=-1).astype(v.dtype)
        o = jnp.einsum('bthgn,btnhd->bthgd', pr, v_g)
        return o.reshape(B, Q_BLOCK, ATTN_WIDTH)

    out = lax.map(block, (q_b, qi_b, wi_b, t_b))
    return jnp.moveaxis(out, 0, 1).reshape(B, S, ATTN_WIDTH)


def conformer_conv(u, conv_dw, conv_b, ln_g, ln_b, conv_pw):
    a, g = jnp.split(u, 2, axis=-1)
    y = a * jax.nn.sigmoid(g)
    y = lax.conv_general_dilated(
        y, conv_dw[:, None, :], window_strides=(1,), padding=[(CONV_KERNEL - 1, 0)],
        dimension_numbers=('NWC', 'WIO', 'NWC'), feature_group_count=CONV_WIDTH)
    y = y + conv_b
    y = jax.nn.silu(layernorm(y, ln_g, ln_b))
    return jnp.einsum('bsc,cd->bsd', y, conv_pw)


def multiscale_pool(u, pool_w, pool_scale):
    B, S, _ = u.shape
    uf = u.astype(jnp.float32)
    c = jnp.cumsum(uf, axis=1)
    c_pad = jnp.concatenate([jnp.zeros((B, POOL_PAD, POOL_WIDTH), jnp.float32), c], axis=1)
    t = jnp.arange(S)
    outs = []
    for gi, w in enumerate(POOL_WINDOWS):
        sl = slice(gi * POOL_GROUP_DIM, (gi + 1) * POOL_GROUP_DIM)
        win_sum = c[:, :, sl] - c_pad[:, POOL_PAD - w:POOL_PAD - w + S, sl]
        count = jnp.minimum(t + 1, w).astype(jnp.float32)[None, :, None]
        outs.append(win_sum / count - uf[:, :, sl])
    y = jnp.stack(outs, axis=2).astype(u.dtype)
    y = jnp.einsum('bsgc,gcd->bsgd', y, pool_w).reshape(B, S, POOL_WIDTH)
    return y * pool_scale


def setup_inputs(seed: int = 0) -> dict:
    key = jax.random.key(seed)
    ks = jax.random.split(key, 24)
    nrm = jax.random.normal
    f32 = jnp.float32
    L = DEPTH
    x = nrm(ks[0], (BATCH, SEQ, D_MODEL), f32)
    p = nrm(ks[1], (DEPTH, BATCH, SEQ, PLE_DIM), f32)
    offs = jax.random.randint(ks[2], (BATCH, 1), 0, MAX_POS_OFFSET, dtype=jnp.int32)
    positions = (offs + jnp.arange(SEQ, dtype=jnp.int32)[None, :]).astype(jnp.int32)
    return {
        "x": x,
        "p": p,
        "positions": positions,
        "norm_mix_pre": 1.0 + 0.1 * nrm(ks[3], (L, D_MODEL), f32),
        "w_in": nrm(ks[4], (L, D_MODEL, IN_WIDTH), f32) * D_MODEL ** -0.5,
        "conv_dw": nrm(ks[5], (L, CONV_KERNEL, CONV_WIDTH), f32) * CONV_KERNEL ** -0.5,
        "conv_b": 0.02 * nrm(ks[6], (L, CONV_WIDTH), f32),
        "conv_ln_g": 1.0 + 0.1 * nrm(ks[7], (L, CONV_WIDTH), f32),
        "conv_ln_b": 0.02 * nrm(ks[8], (L, CONV_WIDTH), f32),
        "conv_pw": nrm(ks[9], (L, CONV_WIDTH, CONV_WIDTH), f32) * CONV_WIDTH ** -0.5,
        "pool_w": nrm(ks[10], (L, POOL_GROUPS, POOL_GROUP_DIM, POOL_GROUP_DIM), f32) * POOL_GROUP_DIM ** -0.5,
        "pool_scale": 1.0 + 0.1 * nrm(ks[11], (L, POOL_WIDTH), f32),
        "w_out": nrm(ks[12], (L, MIX_WIDTH, D_MODEL), f32) * MIX_WIDTH ** -0.5,
        "norm_mix_post": 1.0 + 0.1 * nrm(ks[13], (L, D_MODEL), f32),
        "norm_mlp_pre": 1.0 + 0.1 * nrm(ks[14], (L, D_MODEL), f32),
        "w_up": nrm(ks[15], (L, D_MODEL, D_FF), f32) * D_MODEL ** -0.5,
        "w_down": nrm(ks[16], (L, D_FF, D_MODEL), f32) * D_FF ** -0.5,
        "norm_mlp_post": 1.0 + 0.1 * nrm(ks[17], (L, D_MODEL), f32),
        "ple_proj": nrm(ks[18], (L, PLE_DIM, D_MODEL), f32) * PLE_DIM ** -0.5,
        "ple_gate": nrm(ks[19], (L, D_MODEL, D_MODEL), f32) * D_MODEL ** -0.5,
    }


def reference(x, p, positions, norm_mix_pre, w_in, conv_dw, conv_b, conv_ln_g, conv_ln_b,
              conv_pw, pool_w, pool_scale, w_out, norm_mix_post, norm_mlp_pre, w_up, w_down,
              norm_mlp_post, ple_proj, ple_gate):
    B, S, _ = x.shape
    cos, sin = rope_tables(positions)
    h = x
    for i in range(DEPTH):
        a = rmsnorm(h, norm_mix_pre[i])
        u = jnp.einsum('bsd,de->bse', a, w_in[i])
        q, k, v, qi, ki, wi, u_conv, u_pool = jnp.split(u, IN_SPLITS, axis=-1)
        q = partial_rope(q.reshape(B, S, ATTN_HEADS, HEAD_DIM), cos, sin)
        k = partial_rope(k.reshape(B, S, KV_HEADS, HEAD_DIM), cos, sin)
        v = v.reshape(B, S, KV_HEADS, HEAD_DIM)
        qi = partial_rope(qi.reshape(B, S, IDX_HEADS, IDX_DIM), cos, sin)
        ki = partial_rope(ki[:, :, None, :], cos, sin)[:, :, 0, :]
        y_attn = dsa_sparse_attention(q, k, v, qi, ki, wi)
        y_conv = conformer_conv(u_conv, conv_dw[i], conv_b[i], conv_ln_g[i],
                                conv_ln_b[i], conv_pw[i])
        y_pool = multiscale_pool(u_pool, pool_w[i], pool_scale[i])
        mix = jnp.concatenate([y_attn, y_conv, y_pool], axis=-1)
        mix = jnp.einsum('bse,ed->bsd', mix, w_out[i])
        h = h + rmsnorm(mix, norm_mix_post[i])
        m = rmsnorm(h, norm_mlp_pre[i])
        m = jnp.square(jax.nn.relu(jnp.einsum('bsd,df->bsf', m, w_up[i])))
        m = jnp.einsum('bsf,fd->bsd', m, w_down[i])
        h = h + rmsnorm(m, norm_mlp_post[i])
        gate = jax.nn.sigmoid(jnp.einsum('bsd,de->bse', h, ple_gate[i]))
        h = h + gate * jnp.einsum('bsp,pd->bsd', p[i], ple_proj[i])
    return h
```

```python
import math
from contextlib import ExitStack

import numpy as np
import concourse.bass as bass
import concourse.mybir as mybir
from concourse.bass_utils import run_bass_kernel_spmd

F32 = mybir.dt.float32
BF16 = mybir.dt.bfloat16
I32 = mybir.dt.int32
ALU = mybir.AluOpType
AF = mybir.ActivationFunctionType
AX = mybir.AxisListType

S = 2048
D = 1024
NT = 16
INW = 2640
TMW = 1872
CPW = 768
DFF = 4096
NIT = 18
TOPK = 256
NEG = -30000.0
ATT_SCALE = 0.125
NORM_EPS = 1e-6
LN_EPS = 1e-5
ARENA_KB = 134


class Res:
    __slots__ = ("name", "w", "r")

    def __init__(self, name):
        self.name = name
        self.w = None
        self.r = {}


class Prog:
    ENGS = ("pe", "act", "dve", "pool", "sp")

    def __init__(self, nc, stack):
        self.nc = nc
        self.stack = stack
        self.q = {e: [] for e in self.ENGS}
        self.cnt = {e: 0 for e in self.ENGS}
        self.known = {e: {} for e in self.ENGS}
        self.sems = {}
        self.dcnt = {}
        self.resources = {}
        for e in self.ENGS:
            self.sems[e] = stack.enter_context(nc.semaphore("s_" + e))
        self.pe_pending = False

    def res(self, name):
        r = self.resources.get(name)
        if r is None:
            r = Res(name)
            self.resources[name] = r
        return r

    def _sem(self, key):
        s = self.sems.get(key)
        if s is None:
            s = self.stack.enter_context(self.nc.semaphore("s_" + key.replace(":", "_")))
            self.sems[key] = s
        return s

    def _need(self, eng, tok):
        if tok is None:
            return
        key, val = tok
        if eng == "pe" and key == "pe":
            return
        if self.known[eng].get(key, 0) >= val:
            return
        self.known[eng][key] = val
        self.q[eng].append(("w", key, val))

    def _deps(self, eng, R, W):
        for r in R:
            r = self.res(r) if isinstance(r, str) else r
            self._need(eng, r.w)
        for w in W:
            w = self.res(w) if isinstance(w, str) else w
            self._need(eng, w.w)
            for k, v in w.r.items():
                self._need(eng, (k, v))

    def _update(self, tok, R, W):
        for r in R:
            r = self.res(r) if isinstance(r, str) else r
            if r.r.get(tok[0], 0) < tok[1]:
                r.r[tok[0]] = tok[1]
        for w in W:
            w = self.res(w) if isinstance(w, str) else w
            w.w = tok
            w.r = {}

    def record(self, fn, *args):
        self.rec = [[]]
        fn(*args)
        chunks = [c for c in self.rec if c]
        self.rec = None
        return chunks

    def mark(self):
        if self.rec is not None and self.rec[-1]:
            self.rec.append([])

    def replay_merged(self, lists):
        tot = [sum(len(c) for c in l) for l in lists]
        pos = [0] * len(lists)
        done = [0] * len(lists)
        while True:
            best = None
            for i, l in enumerate(lists):
                if pos[i] < len(l):
                    fr = done[i] / max(1, tot[i])
                    if best is None or fr < best[0]:
                        best = (fr, i)
            if best is None:
                break
            i = best[1]
            for ent in lists[i][pos[i]]:
                if ent[0] == "op":
                    self.op(*ent[1:])
                else:
                    self.dma(*ent[1:])
            done[i] += len(lists[i][pos[i]])
            pos[i] += 1

    def op(self, eng, fn, R=(), W=(), sig=True):
        if getattr(self, "rec", None) is not None:
            self.rec[-1].append(("op", eng, fn, tuple(R), tuple(W), sig))
            return
        self._deps(eng, R, W)
        if sig:
            self.cnt[eng] += 1
            tok = (eng, self.cnt[eng])
            self.q[eng].append(("i", fn, [(eng, 1)]))
            if eng == "pe":
                self.pe_pending = False
        else:
            assert eng == "pe"
            tok = (eng, self.cnt[eng] + 1)
            self.q[eng].append(("i", fn, []))
            self.pe_pending = True
        self._update(tok, R, W)

    def dma(self, qeng, fn, R=(), W=(), sem=None):
        if getattr(self, "rec", None) is not None:
            self.rec[-1].append(("dma", qeng, fn, tuple(R), tuple(W), sem))
            return
        key = "d:" + sem
        for r in R:
            self._need(qeng, self.res(r).w)
        for w in W:
            w = self.res(w)
            if not (w.w is not None and w.w[0] == key):
                self._need(qeng, w.w)
            for k, v in w.r.items():
                self._need(qeng, (k, v))
        self._sem(key)
        self.dcnt[key] = self.dcnt.get(key, 0) + 16
        tok = (key, self.dcnt[key])
        self.q[qeng].append(("i", fn, [(key, 16)]))
        self._update(tok, R, W)

    def group_done(self, sem, names):
        key = "d:" + sem
        for n in names:
            self.res(n).w = (key, self.dcnt[key])

    def barrier(self):
        assert not self.pe_pending
        for e in self.ENGS:
            for o in self.ENGS:
                if o != e and self.cnt[o] > 0:
                    self._need(e, (o, self.cnt[o]))
            for key, v in self.dcnt.items():
                self._need(e, (key, v))
        for r in self.resources.values():
            r.w = None
            r.r = {}
        self.flush()

    def flush(self):
        if not any(self.q[e] for e in self.ENGS):
            return
        with self.nc.Block() as block:
            self.emit(block)
        self.q = {e: [] for e in self.ENGS}

    def emit(self, block):
        nc = self.nc
        engobj = {"pe": nc.tensor, "act": nc.scalar, "dve": nc.vector, "pool": nc.gpsimd, "sp": nc.sync}
        deco = {"pe": block.tensor, "act": block.scalar, "dve": block.vector, "pool": block.gpsimd,
                "sp": block.sync}
        for e in self.ENGS:
            lst = self.q[e]
            eo = engobj[e]

            def body(_eng, lst=lst, eo=eo):
                for ent in lst:
                    if ent[0] == "w":
                        eo.wait_ge(self.sems[ent[1]], ent[2])
                    else:
                        ins = ent[1](eo)
                        for key, amt in ent[2]:
                            ins = ins.then_inc(self.sems[key], amt)

            deco[e](body)


class Arena:
    def __init__(self, ap, nelem):
        self.ap = ap
        self.n = nelem
        self.off = 0

    def reset(self):
        self.off = 0

    def bf(self, shape):
        n = int(np.prod(shape[1:]))
        n = (n + 1) // 2 * 2
        v = self.ap[:, self.off:self.off + n]
        self.off += n
        assert self.off <= self.n, ("arena overflow", self.off, self.n)
        return self._shape(v, shape)

    def f32(self, shape):
        n = int(np.prod(shape[1:])) * 2
        v = self.ap[:, self.off:self.off + n].bitcast(F32)
        self.off += n
        assert self.off <= self.n, ("arena overflow", self.off, self.n)
        return self._shape(v, shape)

    @staticmethod
    def _shape(v, shape):
        if len(shape) == 2:
            return v
        if len(shape) == 3:
            return v.rearrange("p (a b) -> p a b", a=shape[1])
        if len(shape) == 4:
            return v.rearrange("p (a b c) -> p a b c", a=shape[1], b=shape[2])
        raise ValueError(shape)


def build_program(nl, dbg=False):
    nc = bass.Bass("TRN2", target_bir_lowering=False)

    def din(name, shape, dt=F32):
        return nc.dram_tensor(name, list(shape), dt, kind="ExternalInput").ap()

    x_d = din("x", [S, D])
    p_d = din("p", [nl, S, 256])
    pos_d = din("post", [128, NT], I32)
    invf_d = din("invf", [128, 8])
    bcn_d = din("bconst", [128, 2 * NIT])
    rcc_d = din("rcc", [128, 2, 16])
    gpre_d = din("gpre", [nl, 128, 8])
    gmlp_d = din("gmlp", [nl, 128, 8])
    gpost_d = din("gpost", [nl, 1, D])
    gmpost_d = din("gmpost", [nl, 1, D])
    win_d = din("w_in", [nl, D, INW])
    dw_d = din("dwt", [nl, 128, 2, 31])
    sm_d = din("smallp", [nl, 128, 8])
    cpw_d = din("conv_pw", [nl, 256, 256])
    plw_d = din("pool_w", [nl, 4, 64, 64])
    wout_d = din("w_out", [nl, D, D])
    wup_d = din("w_up", [nl, D, DFF])
    wdn_d = din("w_down", [nl, DFF, D])
    wpl_d = din("ple_proj", [nl, 256, D])
    wg_d = din("ple_gate", [nl, D, D])
    out_d = nc.dram_tensor("out", [S, D], F32, kind="ExternalOutput").ap()
    dbg_d = None
    if dbg:
        dbg_d = nc.dram_tensor("dbg", [128, 40000], F32, kind="ExternalOutput").ap()

    with ExitStack() as st:
        def sb(name, shape, dt=F32):
            return st.enter_context(nc.sbuf_tensor(name, list(shape), dt))

        def ps(name, shape, dt=F32):
            return st.enter_context(nc.psum_tensor(name, list(shape), dt))

        h = sb("h", [128, NT, D])
        arena_t = sb("arena", [128, ARENA_KB * 512], BF16)
        identf = sb("identf", [128, 128])
        identb = sb("identb", [128, 128], BF16)
        identb4 = sb("identb4", [128, 4, 128], BF16)
        cmask = sb("cmask", [128, 128])
        onesm = sb("onesm", [128, 128])
        cosT = sb("cosT", [128, NT, 8])
        sinT = sb("sinT", [128, NT, 8])
        posi = sb("posi", [128, NT], I32)
        posf = sb("posf", [128, NT])
        invf = sb("invf_s", [128, 8])
        ang = sb("ang", [128, NT, 8])
        ang2 = sb("ang2", [128, NT, 8])
        angi = sb("angi", [128, NT, 8], I32)
        bcn = sb("bcn", [128, 2 * NIT])
        rcc = sb("rcc_s", [128, 2, 16])
        gpre = sb("gpre_s", [128, 8])
        gmlp = sb("gmlp_s", [128, 8])
        smp = sb("smp", [128, 8])
        dwc = sb("dwc", [128, 2, 31])
        epsn = sb("epsn", [128, 1])
        epsl = sb("epsl", [128, 1])
        stat = sb("stat", [128, 64])
        rstd_all = sb("rstd_all", [128, NT])
        bis = sb("bis", [128, 4 * NIT + 16])
        dscr = sb("dscr", [128, 2048]) if dbg else None

        psT = ps("psT", [128, 1024])
        psU = ps("psU", [128, 2048])
        psF = ps("psF", [128, 1024])

        P = Prog(nc, st)
        A = Arena(arena_t, ARENA_KB * 512)

        def V(fn, R=(), W=()):
            P.op("dve", fn, R, W)

        def AC(fn, R=(), W=()):
            P.op("act", fn, R, W)

        def G(fn, R=(), W=()):
            P.op("pool", fn, R, W)

        def MM(out, lhsT, rhs, start, stop, R=(), W=(), sig=None):
            if sig is None:
                sig = stop
            P.op("pe", lambda e: e.matmul(out, lhsT=lhsT, rhs=rhs, start=start, stop=stop), R, W, sig=sig)

        def TP(out, in_, ident, R=(), W=(), sig=True):
            P.op("pe", lambda e: e.transpose(out, in_, ident), R, W, sig=sig)

        def rstd_from_ssq(ssq_ap, out_ap, n, eps_ap, rname, wname):
            AC(lambda e: e.activation(out=out_ap, in_=ssq_ap, func=AF.Sqrt, bias=eps_ap, scale=1.0 / n),
               R=[rname], W=[wname])
            V(lambda e: e.reciprocal(out=out_ap, in_=out_ap), R=[wname], W=[wname])

        for t in range(NT):
            P.dma("sp", lambda e, t=t: e.dma_start(out=h[:, t, :], in_=x_d[t * 128:(t + 1) * 128, :]),
                  W=["h%d" % t], sem="hload")
        P.group_done("hload", ["h%d" % t for t in range(NT)])
        P.dma("sp", lambda e: e.dma_start(out=posi[:], in_=pos_d), W=["posi"], sem="c0")
        P.dma("sp", lambda e: e.dma_start(out=invf[:], in_=invf_d), W=["invf"], sem="c0")
        P.dma("sp", lambda e: e.dma_start(out=bcn[:], in_=bcn_d), W=["bcn"], sem="c0")
        P.dma("sp", lambda e: e.dma_start(out=rcc[:], in_=rcc_d), W=["rcc"], sem="c0")
        P.group_done("c0", ["posi", "invf", "bcn", "rcc"])

        G(lambda e: e.memset(identf[:], 0.0), W=["identf"])
        G(lambda e: e.affine_select(out=identf[:], in_=identf[:], pattern=[[-1, 128]], compare_op=ALU.not_equal,
                                    fill=1.0, base=0, channel_multiplier=1), R=["identf"], W=["identf"])
        G(lambda e: e.tensor_copy(out=identb[:], in_=identf[:]), R=["identf"], W=["identb"])
        for c4_ in range(4):
            G(lambda e, c4_=c4_: e.tensor_copy(out=identb4[:, c4_, :], in_=identf[:]), R=["identf"], W=["identb4"])
        G(lambda e: e.memset(cmask[:], 0.0), W=["cmask"])
        G(lambda e: e.affine_select(out=cmask[:], in_=cmask[:], pattern=[[-1, 128]], compare_op=ALU.is_ge,
                                    fill=-1e30, base=0, channel_multiplier=1), R=["cmask"], W=["cmask"])
        G(lambda e: e.memset(onesm[:], 1.0 / 256.0), W=["onesm"])
        G(lambda e: e.memset(epsn[:], NORM_EPS), W=["epsn"])
        G(lambda e: e.memset(epsl[:], LN_EPS), W=["epsl"])

        V(lambda e: e.tensor_copy(out=posf[:], in_=posi[:]), R=["posi"], W=["posf"])
        V(lambda e: e.tensor_tensor(out=ang[:], in0=posf[:].unsqueeze(2).to_broadcast([128, NT, 8]),
                                    in1=invf[:].unsqueeze(1).to_broadcast([128, NT, 8]), op=ALU.mult),
          R=["posf", "invf"], W=["ang"])
        for (tab, off, nm) in ((sinT, 0.0, "sinT"), (cosT, 0.25, "cosT")):
            V(lambda e, off=off: e.tensor_scalar(out=ang2[:], in0=ang[:], scalar1=1.0 / (2 * math.pi), scalar2=off,
                                                 op0=ALU.mult, op1=ALU.add), R=["ang"], W=["ang2"])
            V(lambda e: e.tensor_copy(out=angi[:], in_=ang2[:]), R=["ang2"], W=["angi"])
            V(lambda e, tab=tab: e.tensor_copy(out=tab[:], in_=angi[:]), R=["angi"], W=[nm])
            V(lambda e, tab=tab: e.tensor_tensor(out=ang2[:], in0=ang2[:], in1=tab[:], op=ALU.subtract),
              R=["ang2", nm], W=["ang2"])
            AC(lambda e, tab=tab: e.activation(out=tab[:], in_=ang2[:], func=AF.Sin, scale=6.2831),
               R=["ang2"], W=[nm])
        P.barrier()

        dbg_slot = [0]

        def dump(ap_f32_2d, ncols, rname):
            if not dbg:
                return
            print("dump", rname, dbg_slot[0], ncols)
            if ap_f32_2d.dtype == F32:
                o = dbg_slot[0]
                P.dma("sp", lambda e: e.dma_start(out=dbg_d[:, o:o + ncols], in_=ap_f32_2d), R=[rname], sem="dbg%d" % o)
                dbg_slot[0] += ncols
                return
            for c0 in range(0, ncols, 2048):
                n = min(2048, ncols - c0)
                o = dbg_slot[0]
                V(lambda e, c0=c0, n=n: e.tensor_copy(out=dscr[:, 0:n], in_=ap_f32_2d[:, c0:c0 + n]),
                  R=[rname], W=["dscr"])
                P.dma("sp", lambda e, o=o, n=n: e.dma_start(out=dbg_d[:, o:o + n], in_=dscr[:, 0:n]),
                      R=["dscr"], sem="dbgs")
                dbg_slot[0] += n

        dump(cosT[:].rearrange("p a b -> p (a b)"), 128, "cosT")
        dump(sinT[:].rearrange("p a b -> p (a b)"), 128, "sinT")
        try:
          for L in range(nl):
              P.dma("sp", lambda e, L=L: e.dma_start(out=gpre[:], in_=gpre_d[L]), W=["gpre"], sem="c1")
              P.dma("sp", lambda e, L=L: e.dma_start(out=gmlp[:], in_=gmlp_d[L]), W=["gmlp"], sem="c1")
              P.dma("sp", lambda e, L=L: e.dma_start(out=smp[:], in_=sm_d[L]), W=["smp"], sem="c1")
              P.dma("sp", lambda e, L=L: e.dma_start(out=dwc[:], in_=dw_d[L]), W=["dwc"], sem="c1")
              P.group_done("c1", ["gpre", "gmlp", "smp", "dwc"])

              A.reset()
              mixT = A.bf([128, 8, S])
              wbig = A.bf([128, 8, TMW])
              kT = A.bf([128, S])
              kiT = A.bf([128, S])
              vext = A.bf([128, NT, 2, 65])
              aT = A.bf([128, 8, 128])
              qT = [A.bf([128, 4, 128]) for _ in range(4)]
              qiT = [A.bf([128, 8, 128]) for _ in range(2)]
              pT = [A.bf([128, 4, 128]) for _ in range(2)]
              mb = [A.bf([128, S]) for _ in range(2)]
              accs = [A.f32([128, S]) for _ in range(2)]
              hn = A.f32([128, D])
              utm = A.f32([128, TMW])
              kidup = A.f32([128, 128])
              rbuf = [A.f32([128, 512]) for _ in range(2)]
              ytm = A.f32([128, 512])
              rtA = A.f32([128, 17, 16])
              rtB = A.f32([128, 17, 16])
              qpair = A.f32([128, 512])
              wiS = [A.f32([128, 16]) for _ in range(2)]

              for c in range(8):
                  P.dma("pool", lambda e, L=L, c=c: e.dma_start(out=wbig[:, c, :],
                                                                 in_=win_d[L, c * 128:(c + 1) * 128, 0:TMW]),
                        W=["wbig"], sem="wbig")
              G(lambda e: e.memset(vext[:, :, :, 64:65], 1.0), W=["vx%d" % t for t in range(NT)])

              bTP = (psT[:, 0:512], "psT0")
              bU = [(psT[:, 512:1024], "psT1"), (psF[:, 0:512], "psF0")]
              bI = [(psU[:, 0:512], "psU0"), (psU[:, 512:1024], "psU1")]
              bS = [(psU[:, 1024:1536], "psU2"), (psU[:, 1536:2048], "psU3")]
              bPV = (psF[:, 512:1024], "psF1")

              def stageA(T):
                  hT_ = "h%d" % T
                  ts = slice(T * 128, (T + 1) * 128)
                  AC(lambda e: e.activation(out=hn[:], in_=h[:, T, :], func=AF.Square,
                                            accum_out=stat[:, 0:1]), R=[hT_], W=["hn", "st0"])
                  rstd_from_ssq(stat[:, 0:1], rstd_all[:, T:T + 1], D, epsn[:], "st0", "rstd%d" % T)
                  V(lambda e: e.tensor_scalar(out=hn[:], in0=h[:, T, :], scalar1=rstd_all[:, T:T + 1],
                                              scalar2=None, op0=ALU.mult), R=[hT_, "rstd%d" % T], W=["hn"])
                  P.mark()
                  for hf in range(2):
                      for c4 in range(4):
                          c = hf * 4 + c4
                          TP(bTP[0][:, c4 * 128:(c4 + 1) * 128], hn[:, c * 128:(c + 1) * 128], identf[:],
                             R=["hn"], W=[bTP[1]], sig=(c4 == 3))
                      V(lambda e, hf=hf: e.tensor_tensor(
                          out=aT[:, hf * 4:(hf + 1) * 4, :], in0=bTP[0].rearrange("p (c t) -> p c t", c=4),
                          in1=gpre[:, hf * 4:(hf + 1) * 4].unsqueeze(2).to_broadcast([128, 4, 128]), op=ALU.mult),
                        R=[bTP[1], "gpre"], W=["aT"])
                      P.mark()
                  cbs = [(0, 512), (512, 1024), (1024, 1536), (1536, TMW)]
                  for bi, (c0, c1) in enumerate(cbs):
                      bk, bn = bU[bi % 2]
                      for c in range(8):
                          MM(bk[:, 0:(c1 - c0)], aT[:, c, :], wbig[:, c, c0:c1],
                             start=(c == 0), stop=(c == 7), R=["aT", "wbig"], W=[bn])
                      AC(lambda e, bk=bk, c0=c0, c1=c1: e.copy(out=utm[:, c0:c1], in_=bk[:, 0:(c1 - c0)]),
                         R=[bn], W=["utm%d" % bi])
                      P.mark()
                  UT = ["utm0", "utm1", "utm2", "utm3"]
                  for (c0, nh) in ((0, 10), (768, 17)):
                      xv = utm[:, c0:c0 + nh * 64].rearrange("p (h d) -> p h d", h=nh)
                      x12 = xv[:, :, 0:16]
                      cosb = cosT[:, T:T + 1, :].to_broadcast([128, nh, 8])
                      sinb = sinT[:, T:T + 1, :].to_broadcast([128, nh, 8])
                      a_ = rtA[:, 0:nh, :]
                      b_ = rtB[:, 0:nh, :]
                      for half in range(2):
                          hs = slice(half * 8, half * 8 + 8)
                          V(lambda e, hs=hs, a_=a_, x12=x12, cosb=cosb: e.tensor_tensor(
                              out=a_[:, :, hs], in0=x12[:, :, hs], in1=cosb, op=ALU.mult), R=UT, W=["rtA"])
                          V(lambda e, hs=hs, b_=b_, x12=x12, sinb=sinb: e.tensor_tensor(
                              out=b_[:, :, hs], in0=x12[:, :, hs], in1=sinb, op=ALU.mult), R=UT, W=["rtB"])
                      V(lambda e, a_=a_, b_=b_, x12=x12: e.tensor_tensor(
                          out=x12[:, :, 0:8], in0=a_[:, :, 0:8], in1=b_[:, :, 8:16], op=ALU.subtract),
                        R=["rtA", "rtB"], W=UT)
                      V(lambda e, a_=a_, b_=b_, x12=x12: e.tensor_tensor(
                          out=x12[:, :, 8:16], in0=a_[:, :, 8:16], in1=b_[:, :, 0:8], op=ALU.add),
                        R=["rtA", "rtB"], W=UT)
                      P.mark()
                  V(lambda e: e.tensor_copy(out=kidup[:].rearrange("p (a d) -> p a d", a=2),
                                            in_=utm[:, 1792:1856].unsqueeze(1).to_broadcast([128, 2, 64])),
                    R=UT, W=["kidup"])
                  V(lambda e: e.tensor_copy(out=vext[:, T, :, 0:64],
                                            in_=utm[:, 640:768].rearrange("p (g d) -> p g d", g=2)),
                    R=UT, W=["vx%d" % T])
                  V(lambda e: e.tensor_copy(out=wiS[T % 2][:], in_=utm[:, 1856:1872]), R=UT, W=["wiS%d" % (T % 2)])
                  V(lambda e: e.tensor_copy(out=qpair[:].rearrange("p (c g d) -> p c g d", c=4, g=2),
                                            in_=utm[:, 0:512].rearrange("p (g c d) -> p c g d", g=2, c=4)),
                    R=UT, W=["qpair"])
                  P.mark()
                  qi_ = qiT[T % 2]
                  for hf in range(2):
                      for c4 in range(4):
                          c = hf * 4 + c4
                          TP(bTP[0][:, c4 * 128:(c4 + 1) * 128], utm[:, 768 + c * 128: 768 + (c + 1) * 128],
                             identf[:], R=UT, W=[bTP[1]], sig=(c4 == 3))
                      AC(lambda e, hf=hf, qi_=qi_: e.copy(out=qi_[:, hf * 4:(hf + 1) * 4, :],
                                                          in_=bTP[0].rearrange("p (c t) -> p c t", c=4)),
                         R=[bTP[1]], W=["qiT%d" % (T % 2)])
                      P.mark()
                  for c in range(4):
                      TP(bTP[0][:, c * 128:(c + 1) * 128], qpair[:, c * 128:(c + 1) * 128], identf[:], R=["qpair"],
                         W=[bTP[1]], sig=(c == 3))
                  AC(lambda e: e.copy(out=qT[T % 4][:], in_=bTP[0].rearrange("p (c t) -> p c t", c=4)),
                     R=[bTP[1]], W=["qT%d" % (T % 4)])
                  P.mark()
                  TP(bTP[0][:, 0:128], utm[:, 512:640], identf[:], R=UT, W=[bTP[1]], sig=False)
                  TP(bTP[0][:, 128:256], kidup[:], identf[:], R=["kidup"], W=[bTP[1]], sig=True)
                  AC(lambda e: e.copy(out=kT[:, ts], in_=bTP[0][:, 0:128]), R=[bTP[1]], W=["kT%d" % T])
                  AC(lambda e: e.copy(out=kiT[:, ts], in_=bTP[0][:, 128:256]), R=[bTP[1]], W=["kiT%d" % T])
                  P.mark()

              def stageI(T):
                  ns = T + 1
                  SS = ns * 128
                  nb = (SS + 511) // 512
                  qi_ = qiT[T % 2]
                  qn = "qiT%d" % (T % 2)
                  wi_ = wiS[T % 2]
                  wn = "wiS%d" % (T % 2)
                  acc = accs[T % 2]
                  accn = "acc%d" % (T % 2)
                  it = 0
                  for sbk in range(nb):
                      w = min(512, SS - sbk * 512)
                      cs = slice(sbk * 512, sbk * 512 + w)
                      kin = ["kiT%d" % j for j in range(sbk * 4, min(ns, sbk * 4 + 4))]
                      for hi in range(16):
                          c, hp = hi // 2, hi % 2
                          pr = slice(hp * 64, hp * 64 + 64)
                          bank = it % 2
                          it += 1
                          pso = bI[bank][0][:, 0:w]
                          bn = bI[bank][1]
                          MM(pso, qi_[pr, c, :], kiT[pr, cs], start=True, stop=True, R=[qn] + kin, W=[bn])
                          rb = rbuf[bank][:, 0:w]
                          AC(lambda e, rb=rb, pso=pso: e.activation(out=rb, in_=pso, func=AF.Relu),
                             R=[bn], W=["rb%d" % bank])
                          if hi == 0:
                              V(lambda e, rb=rb, cs=cs: e.tensor_scalar(out=acc[:, cs], in0=rb, scalar1=wi_[:, 0:1],
                                                                         scalar2=None, op0=ALU.mult),
                                R=["rb%d" % bank, wn], W=[accn])
                          else:
                              V(lambda e, rb=rb, cs=cs, hi=hi: e.scalar_tensor_tensor(
                                  out=acc[:, cs], in0=rb, scalar=wi_[:, hi:hi + 1], in1=acc[:, cs],
                                  op0=ALU.mult, op1=ALU.add), R=["rb%d" % bank, wn, accn], W=[accn])
                          P.mark()

              def stageBis(T):
                  ts = slice(T * 128, (T + 1) * 128)
                  ns = T + 1
                  SS = ns * 128
                  acc = accs[T % 2]
                  accn = "acc%d" % (T % 2)
                  mb_ = mb[T % 2]
                  mbn = "mb%d" % (T % 2)
                  junk = mb_
                  thr = bis[:, 0:1]
                  if T >= 2:
                      mx, mn, w0, mid, cntc, tt = (bis[:, 1:2], bis[:, 2:3], bis[:, 3:4], bis[:, 4:5], bis[:, 5:6],
                                                   bis[:, 6:7])
                      av = bis[:, 16:16 + NIT]
                      bv = bis[:, 16 + NIT:16 + 2 * NIT]
                      V(lambda e: e.tensor_reduce(out=mx, in_=acc[:, 0:SS], axis=AX.X, op=ALU.max),
                        R=[accn], W=["b_mx"])
                      V(lambda e: e.tensor_reduce(out=mn, in_=acc[:, 0:SS], axis=AX.X, op=ALU.min),
                        R=[accn], W=["b_mn"])
                      V(lambda e: e.tensor_tensor(out=w0, in0=mx, in1=mn, op=ALU.subtract),
                        R=["b_mx", "b_mn"], W=["b_w0"])
                      V(lambda e: e.tensor_scalar(out=av, in0=bcn[:, 0:NIT], scalar1=w0, scalar2=None, op0=ALU.mult),
                        R=["b_w0", "bcn"], W=["b_av"])
                      V(lambda e: e.tensor_scalar(out=bv, in0=bcn[:, NIT:2 * NIT], scalar1=w0, scalar2=None,
                                                  op0=ALU.mult), R=["b_w0", "bcn"], W=["b_bv"])
                      V(lambda e: e.scalar_tensor_tensor(out=mid, in0=w0, scalar=0.5, in1=mn, op0=ALU.mult,
                                                         op1=ALU.add), R=["b_w0", "b_mn"], W=["b_mid"])
                      P.mark()
                  V(lambda e: e.tensor_tensor(out=acc[:, ts], in0=acc[:, ts], in1=cmask[:], op=ALU.add),
                    R=[accn, "cmask"], W=[accn])
                  if T >= 2:
                      for k in range(NIT):
                          V(lambda e: e.tensor_scalar(out=junk[:, 0:SS], in0=acc[:, 0:SS], scalar1=mid,
                                                      scalar2=0.0, op0=ALU.is_ge, op1=ALU.add, accum_out=cntc),
                            R=[accn, "b_mid"], W=[mbn, "b_cnt"])
                          V(lambda e, k=k: e.tensor_scalar(out=tt, in0=cntc, scalar1=TOPK - 0.5,
                                                           scalar2=av[:, k:k + 1], op0=ALU.is_ge, op1=ALU.mult),
                            R=["b_cnt", "b_av"], W=["b_tt"])
                          dst = mid if k < NIT - 1 else thr
                          V(lambda e, k=k, dst=dst: e.scalar_tensor_tensor(out=dst, in0=tt, scalar=bv[:, k:k + 1],
                                                                           in1=mid, op0=ALU.subtract, op1=ALU.add),
                            R=["b_tt", "b_bv", "b_mid"], W=["b_mid", "b_thr"])
                          P.mark()
                  else:
                      V(lambda e: e.memset(thr, -1e29), W=["b_thr"])
                  V(lambda e: e.tensor_scalar(out=mb_[:, 0:SS], in0=acc[:, 0:SS], scalar1=thr, scalar2=NEG,
                                              op0=ALU.is_lt, op1=ALU.mult), R=[accn, "b_thr"], W=[mbn])
                  P.mark()

              def stageAtt(T):
                  ts = slice(T * 128, (T + 1) * 128)
                  ns = T + 1
                  nsb = (ns + 3) // 4
                  q_ = qT[T % 4]
                  qn = "qT%d" % (T % 4)
                  mb_ = mb[T % 2]
                  mbn = "mb%d" % (T % 2)
                  it2 = 0
                  for grp in range(2):
                      g = grp
                      pr = slice(g * 64, g * 64 + 64)
                      for si in range(ns):
                          sc = slice(si * 128, (si + 1) * 128)
                          sbank, sbn = bS[it2 % 2]
                          pb = it2 % 2
                          it2 += 1
                          MM(sbank, kT[pr, sc], q_[pr, :, :], start=True, stop=False,
                             R=["kT%d" % si, qn], W=[sbn], sig=False)
                          MM(sbank, mb_[:, sc], identb4[:], start=False, stop=True,
                             R=[mbn, "identb4"], W=[sbn], sig=True)
                          AC(lambda e, pb=pb, sbank=sbank: e.activation(
                              out=pT[pb][:], in_=sbank.rearrange("p (j t) -> p j t", j=4),
                              func=AF.Exp, scale=ATT_SCALE), R=[sbn], W=["pT%d" % pb])
                          for h4 in range(4):
                              pvo = bPV[0][:, h4 * 65: h4 * 65 + 65]
                              MM(pvo, pT[pb][:, h4, :], vext[:, si, g, :], start=(si == 0 and h4 == 0), stop=(si == ns - 1),
                                 R=["pT%d" % pb, "vx%d" % si], W=[bPV[1]], sig=(h4 == 3))
                          P.mark()
                      pv = bPV[0][:, 0:260].rearrange("p (h d) -> p h d", h=4)
                      V(lambda e, pv=pv: e.reciprocal(out=stat[:, 8:12].unsqueeze(2), in_=pv[:, :, 64:65]),
                        R=[bPV[1]], W=["rden"])
                      V(lambda e, pv=pv, grp=grp: e.tensor_tensor(
                          out=ytm[:, grp * 256:(grp + 1) * 256].rearrange("p (h d) -> p h d", h=4),
                          in0=pv[:, :, 0:64], in1=stat[:, 8:12].unsqueeze(2).to_broadcast([128, 4, 64]),
                          op=ALU.mult), R=[bPV[1], "rden"], W=["ytm"])
                      P.mark()
                  for c in range(4):
                      TP(bPV[0][:, c * 128:(c + 1) * 128], ytm[:, c * 128:(c + 1) * 128], identf[:],
                         R=["ytm"], W=[bPV[1]], sig=(c == 3))
                  AC(lambda e: e.copy(out=mixT[:, 0:4, ts], in_=bPV[0].rearrange("p (c t) -> p c t", c=4)),
                     R=[bPV[1]], W=["mixT"])
                  P.mark()

              for it_ in range(NT + 3):
                  lists = []
                  if it_ < NT:
                      lists.append(P.record(stageA, it_))
                  if 1 <= it_ <= NT:
                      lists.append(P.record(stageI, it_ - 1))
                  if 2 <= it_ <= NT + 1:
                      lists.append(P.record(stageBis, it_ - 2))
                  if it_ >= 3:
                      lists.append(P.record(stageAtt, it_ - 3))
                  P.replay_merged(lists)
              P.barrier()


              A.reset()
              mixT = A.bf([128, 8, S])
              wcp = A.bf([128, 8, CPW])
              aTb = A.bf([128, 8, 512])
              glu = [A.bf([128, 2, 542]) for _ in range(2)]
              dg = A.bf([128, 62, 128])
              zT = A.bf([128, 2, 512])
              pooled = A.bf([128, 2, 512])
              pww = A.bf([128, 2, 256])
              bd = A.bf([128, 2, 128])
              hn = A.f32([128, D])
              xp = [A.f32([128, 2, 528]) for _ in range(2)]
              yc = A.f32([128, 2, 512])
              ysq = A.f32([128, 2, 512])
              sgm = A.f32([128, 2, 512])
              mean_s = A.f32([128, 512])
              var_s = A.f32([128, 512])
              pa = A.f32([128, 2, 528])
              pb_ = A.f32([128, 2, 528])

              for c in range(8):
                  P.dma("pool", lambda e, L=L, c=c: e.dma_start(out=wcp[:, c, :],
                                                                 in_=win_d[L, c * 128:(c + 1) * 128, TMW:INW]),
                        W=["wcp"], sem="wcp")
              for c in range(2):
                  P.dma("pool", lambda e, L=L, c=c: e.dma_start(out=pww[:, c, :],
                                                                 in_=cpw_d[L, c * 128:(c + 1) * 128, :]),
                        W=["pww"], sem="wsm")
              G(lambda e: e.memset(bd[:], 0.0), W=["bd"])
              for g4 in range(4):
                  r0 = (g4 % 2) * 64
                  P.dma("pool", lambda e, L=L, g4=g4, r0=r0: e.dma_start(out=bd[r0:r0 + 64, g4 // 2, r0:r0 + 64],
                                                                         in_=plw_d[L, g4]),
                        R=[], W=["bd"], sem="wsm")
              P.group_done("wsm", ["pww", "bd"])
              for cc in range(2):
                  for j in range(31):
                      G(lambda e, cc=cc, j=j: e.tensor_scalar(out=dg[:, cc * 31 + j, :], in0=identb[:],
                                                              scalar1=dwc[:, cc, j:j + 1], scalar2=None, op0=ALU.mult),
                        R=["identb", "dwc"], W=["dg"])
              G(lambda e: e.memset(glu[1][:, :, 0:30], 0.0), W=["glu1"])
              G(lambda e: e.memset(xp[1][:], 0.0), W=["xp1"])
              G(lambda e: e.memset(glu[0][:, :, 0:30], 0.0), W=["glu0"])

              for B in range(4):
                  gb, gprev = glu[B % 2], glu[(B + 1) % 2]
                  xb, xprev = xp[B % 2], xp[(B + 1) % 2]
                  gn, gpn = "glu%d" % (B % 2), "glu%d" % ((B + 1) % 2)
                  xn, xpn = "xp%d" % (B % 2), "xp%d" % ((B + 1) % 2)
                  bs = slice(B * 512, (B + 1) * 512)
                  for jt in range(4):
                      T = B * 4 + jt
                      V(lambda e, T=T: e.tensor_scalar(out=hn[:], in0=h[:, T, :], scalar1=rstd_all[:, T:T + 1],
                                                       scalar2=None, op0=ALU.mult), R=["h%d" % T], W=["hn"])
                      for c in range(8):
                          TP(psT[:, c * 128:(c + 1) * 128], hn[:, c * 128:(c + 1) * 128], identf[:],
                             R=["hn"], W=["psT%d" % (c // 4)], sig=(c % 4 == 3))
                      V(lambda e, jt=jt: e.tensor_tensor(out=aTb[:, :, jt * 128:(jt + 1) * 128],
                                                         in0=psT[:].rearrange("p (c t) -> p c t", c=8),
                                                         in1=gpre[:].unsqueeze(2).to_broadcast([128, 8, 128]),
                                                         op=ALU.mult), R=["psT0", "psT1", "gpre"], W=["aTb"])
                  for ch in range(6):
                      bank = ch % 4
                      for c in range(8):
                          MM(psU[:, bank * 512:(bank + 1) * 512], wcp[:, c, ch * 128:(ch + 1) * 128], aTb[:, c, :],
                             start=(c == 0), stop=(c == 7), R=["wcp", "aTb"], W=["psU%d" % bank])
                      if ch in (2, 3):
                          AC(lambda e, ch=ch, bank=bank: e.activation(out=sgm[:, ch - 2, :],
                                                                      in_=psU[:, bank * 512:(bank + 1) * 512],
                                                                      func=AF.Sigmoid),
                             R=["psU%d" % bank], W=["sgm%d" % (ch - 2)])
                      if ch == 3:
                          if B > 0:
                              V(lambda e, gb=gb, gprev=gprev: e.tensor_copy(out=gb[:, :, 0:30],
                                                                            in_=gprev[:, :, 512:542]),
                                R=[gpn], W=[gn])
                          for a2 in range(2):
                              V(lambda e, a2=a2, gb=gb: e.tensor_tensor(out=gb[:, a2, 30:542],
                                                                        in0=psU[:, a2 * 512:(a2 + 1) * 512],
                                                                        in1=sgm[:, a2, :], op=ALU.mult),
                                R=["psU%d" % a2, "sgm%d" % a2], W=[gn])
                      if ch in (4, 5):
                          if ch == 4:
                              if B > 0:
                                  V(lambda e, xb=xb, xprev=xprev: e.tensor_copy(out=xb[:, :, 0:16],
                                                                                in_=xprev[:, :, 512:528]),
                                    R=[xpn], W=[xn])
                              else:
                                  V(lambda e, xb=xb: e.memset(xb[:, :, 0:16], 0.0), W=[xn])
                          AC(lambda e, ch=ch, bank=bank, xb=xb: e.copy(out=xb[:, ch - 4, 16:528],
                                                                       in_=psU[:, bank * 512:(bank + 1) * 512]),
                             R=["psU%d" % bank], W=[xn])
                  for cc in range(2):
                      bank = 2 + cc
                      for j in range(31):
                          MM(psU[:, bank * 512:(bank + 1) * 512], dg[:, cc * 31 + j, :], gb[:, cc, j:j + 512],
                             start=(j == 0), stop=(j == 30), R=["dg", gn], W=["psU%d" % bank])
                      AC(lambda e, cc=cc, bank=bank: e.activation(out=yc[:, cc, :],
                                                                  in_=psU[:, bank * 512:(bank + 1) * 512],
                                                                  func=AF.Identity, bias=smp[:, cc:cc + 1], scale=1.0),
                         R=["psU%d" % bank, "smp"], W=["yc"])
                  AC(lambda e: e.activation(out=ysq[:], in_=yc[:], func=AF.Square), R=["yc"], W=["ysq"])
                  for cc in range(2):
                      MM(psF[:, 0:512], onesm[:], yc[:, cc, :], start=(cc == 0), stop=(cc == 1),
                         R=["onesm", "yc"], W=["psF0"])
                  for cc in range(2):
                      MM(psF[:, 512:1024], onesm[:], ysq[:, cc, :], start=(cc == 0), stop=(cc == 1),
                         R=["onesm", "ysq"], W=["psF1"])
                  AC(lambda e: e.copy(out=mean_s[:], in_=psF[:, 0:512]), R=["psF0"], W=["mean_s"])
                  V(lambda e: e.tensor_tensor(out=var_s[:], in0=mean_s[:], in1=mean_s[:], op=ALU.mult),
                    R=["mean_s"], W=["var_s"])
                  V(lambda e: e.tensor_tensor(out=var_s[:], in0=psF[:, 512:1024], in1=var_s[:], op=ALU.subtract),
                    R=["psF1", "var_s"], W=["var_s"])
                  AC(lambda e: e.activation(out=var_s[:], in_=var_s[:], func=AF.Sqrt, bias=epsl[:], scale=1.0),
                     R=["var_s", "epsl"], W=["var_s"])
                  V(lambda e: e.reciprocal(out=var_s[:], in_=var_s[:]), R=["var_s"], W=["var_s"])
                  for cc in range(2):
                      V(lambda e, cc=cc: e.tensor_tensor(out=yc[:, cc, :], in0=yc[:, cc, :], in1=mean_s[:],
                                                         op=ALU.subtract), R=["yc", "mean_s"], W=["yc"])
                      V(lambda e, cc=cc: e.tensor_tensor(out=yc[:, cc, :], in0=yc[:, cc, :], in1=var_s[:],
                                                         op=ALU.mult), R=["yc", "var_s"], W=["yc"])
                      AC(lambda e, cc=cc: e.activation(out=zT[:, cc, :], in_=yc[:, cc, :], func=AF.Silu,
                                                       bias=smp[:, 4 + cc:5 + cc], scale=smp[:, 2 + cc:3 + cc]),
                         R=["yc", "smp"], W=["zT"])
                  for oc in range(2):
                      for cc in range(2):
                          MM(psF[:, oc * 512:(oc + 1) * 512], pww[:, cc, oc * 128:(oc + 1) * 128], zT[:, cc, :],
                             start=(cc == 0), stop=(cc == 1), R=["pww", "zT"], W=["psF%d" % oc])
                      AC(lambda e, oc=oc, bs=bs: e.copy(out=mixT[:, 4 + oc, bs], in_=psF[:, oc * 512:(oc + 1) * 512]),
                         R=["psF%d" % oc], W=["mixT"])
                  XR = [xn]
                  V(lambda e, xb=xb: e.tensor_tensor(out=pa[:, :, 1:528], in0=xb[:, :, 1:528], in1=xb[:, :, 0:527],
                                                     op=ALU.add), R=XR, W=["pa"])
                  V(lambda e: e.tensor_tensor(out=pb_[:, :, 3:528], in0=pa[:, :, 3:528], in1=pa[:, :, 1:526],
                                              op=ALU.add), R=["pa"], W=["pb"])
                  V(lambda e: e.tensor_tensor(out=pa[:, 1, 7:528], in0=pb_[:, 1, 7:528], in1=pb_[:, 1, 3:524],
                                              op=ALU.add), R=["pb", "pa"], W=["pa"])
                  V(lambda e: e.tensor_tensor(out=pb_[64:128, 1, 15:528], in0=pa[64:128, 1, 15:528],
                                              in1=pa[64:128, 1, 7:520], op=ALU.add), R=["pa", "pb"], W=["pb"])
                  srcs = [(pa, 0, 0, 2), (pb_, 64, 0, 4), (pa, 0, 1, 8), (pb_, 64, 1, 16)]
                  for (src, p0, cc, wdw) in srcs:
                      prt = slice(p0, p0 + 64)
                      V(lambda e, src=src, prt=prt, cc=cc, wdw=wdw, xb=xb: e.scalar_tensor_tensor(
                          out=pooled[prt, cc, :], in0=src[prt, cc, 16:528], scalar=1.0 / wdw, in1=xb[prt, cc, 16:528],
                          op0=ALU.mult, op1=ALU.subtract), R=["pa", "pb", xn], W=["pooled"])
                      if B == 0:
                          V(lambda e, src=src, prt=prt, cc=cc: e.tensor_tensor(
                              out=src[prt, cc, 16:32], in0=src[prt, cc, 16:32], in1=rcc[prt, cc, :], op=ALU.mult),
                            R=["pa", "pb", "rcc"], W=["pa", "pb"])
                          V(lambda e, src=src, prt=prt, cc=cc, xb=xb: e.tensor_tensor(
                              out=pooled[prt, cc, 0:16], in0=src[prt, cc, 16:32], in1=xb[prt, cc, 16:32],
                              op=ALU.subtract), R=["pa", "pb", xn], W=["pooled"])
                  for cc in range(2):
                      MM(psF[:, cc * 512:(cc + 1) * 512], bd[:, cc, :], pooled[:, cc, :], start=True, stop=True,
                         R=["bd", "pooled"], W=["psF%d" % cc])
                      AC(lambda e, cc=cc, bs=bs: e.activation(out=mixT[:, 6 + cc, bs],
                                                              in_=psF[:, cc * 512:(cc + 1) * 512], func=AF.Identity,
                                                              scale=smp[:, 6 + cc:7 + cc]),
                         R=["psF%d" % cc, "smp"], W=["mixT"])
              if L == 0:
                  dump(mixT[:, 4:8, :].rearrange("p a b -> p (a b)"), 4 * S, "mixT")
              P.barrier()

              def post_norm_add(T, gb_ap, gname, tmp):
                  for nbk in range(2):
                      AC(lambda e, nbk=nbk: e.activation(out=tmp[:, nbk * 512:(nbk + 1) * 512],
                                                         in_=psU[:, nbk * 512:(nbk + 1) * 512], func=AF.Square,
                                                         accum_out=stat[:, 16 + nbk:17 + nbk]),
                         R=["psU%d" % nbk], W=["tmp", "ss%d" % nbk])
                  V(lambda e: e.tensor_tensor(out=stat[:, 18:19], in0=stat[:, 16:17], in1=stat[:, 17:18], op=ALU.add),
                    R=["ss0", "ss1"], W=["ss2"])
                  rstd_from_ssq(stat[:, 18:19], stat[:, 19:20], D, epsn[:], "ss2", "ss3")
                  for nbk in range(2):
                      V(lambda e, nbk=nbk: e.scalar_tensor_tensor(out=tmp[:, nbk * 512:(nbk + 1) * 512],
                                                                  in0=psU[:, nbk * 512:(nbk + 1) * 512],
                                                                  scalar=stat[:, 19:20],
                                                                  in1=gb_ap[:, nbk * 512:(nbk + 1) * 512],
                                                                  op0=ALU.mult, op1=ALU.mult),
                        R=["psU%d" % nbk, "ss3", gname], W=["tmp"])
                  G(lambda e, T=T: e.tensor_tensor(out=h[:, T, :], in0=h[:, T, :], in1=tmp[:], op=ALU.add),
                    R=["tmp", "h%d" % T], W=["h%d" % T])

              A.reset()
              mixT = A.bf([128, 8, S])
              wout = A.bf([128, 8, D])
              gpb = A.f32([128, D])
              tmp = A.f32([128, D])
              for c in range(8):
                  P.dma("pool", lambda e, L=L, c=c: e.dma_start(out=wout[:, c, :], in_=wout_d[L, c * 128:(c + 1) * 128, :]),
                        W=["wout"], sem="wout")
              P.dma("sp", lambda e, L=L: e.dma_start(out=gpb[:], in_=gpost_d[L].to_broadcast([128, D])),
                    W=["gpb"], sem="c2")
              for T in range(NT):
                  ts = slice(T * 128, (T + 1) * 128)
                  for nbk in range(2):
                      for c in range(8):
                          MM(psU[:, nbk * 512:(nbk + 1) * 512], mixT[:, c, ts], wout[:, c, nbk * 512:(nbk + 1) * 512],
                             start=(c == 0), stop=(c == 7), R=["mixT", "wout"], W=["psU%d" % nbk])
                  post_norm_add(T, gpb, "gpb", tmp)
              if L == 0:
                  for T in (0, 5, 15):
                      dump(h[:, T, :], D, "h%d" % T)
              P.barrier()

              A.reset()
              mT = A.bf([128, 8, 1024])
              hid = A.bf([128, 32, 1024])
              NSLOT = 4
              slots = [A.bf([128, 4096]) for _ in range(NSLOT)]
              hn = A.f32([128, D])
              sq = [A.f32([128, 512]) for _ in range(2)]
              gpb = A.f32([128, D])
              tmp = A.f32([128, D])
              P.dma("sp", lambda e, L=L: e.dma_start(out=gpb[:], in_=gmpost_d[L].to_broadcast([128, D])),
                    W=["gpb"], sem="c2")
              slot_i = [0]

              def load_slot(src_fn, shape_a):
                  i = slot_i[0] % NSLOT
                  slot_i[0] += 1
                  view = slots[i].rearrange("p (a b) -> p a b", a=shape_a)
                  for a in range(shape_a):
                      P.dma("pool", lambda e, a=a, view=view: e.dma_start(out=view[:, a, :], in_=src_fn(a)),
                            W=["slot%d" % i], sem="slot%d" % i)
                  return view, "slot%d" % i

              for B in range(2):
                  for jt in range(8):
                      T = B * 8 + jt
                      AC(lambda e, T=T: e.activation(out=hn[:], in_=h[:, T, :], func=AF.Square,
                                                     accum_out=stat[:, 0:1]), R=["h%d" % T], W=["hn", "st0"])
                      rstd_from_ssq(stat[:, 0:1], stat[:, 1:2], D, epsn[:], "st0", "st1")
                      V(lambda e, T=T: e.tensor_scalar(out=hn[:], in0=h[:, T, :], scalar1=stat[:, 1:2],
                                                       scalar2=None, op0=ALU.mult), R=["h%d" % T, "st1"], W=["hn"])
                      for c in range(8):
                          TP(psT[:, c * 128:(c + 1) * 128], hn[:, c * 128:(c + 1) * 128], identf[:],
                             R=["hn"], W=["psT%d" % (c // 4)], sig=(c % 4 == 3))
                      V(lambda e, jt=jt: e.tensor_tensor(out=mT[:, :, jt * 128:(jt + 1) * 128],
                                                         in0=psT[:].rearrange("p (c t) -> p c t", c=8),
                                                         in1=gmlp[:].unsqueeze(2).to_broadcast([128, 8, 128]),
                                                         op=ALU.mult), R=["psT0", "psT1", "gmlp"], W=["mT"])
                  it3 = 0
                  for fg in range(8):
                      wv, wn = load_slot(lambda a, L=L, fg=fg: wup_d[L, a * 128:(a + 1) * 128, fg * 512:(fg + 1) * 512], 8)
                      for f in range(4):
                        for th in range(2):
                          bank = it3 % 2
                          it3 += 1
                          po = psF[:, bank * 512:(bank + 1) * 512]
                          for c in range(8):
                              MM(po, wv[:, c, f * 128:(f + 1) * 128], mT[:, c, th * 512:(th + 1) * 512],
                                 start=(c == 0), stop=(c == 7), R=[wn, "mT"], W=["psF%d" % bank])
                          AC(lambda e, po=po, bank=bank: e.activation(out=sq[bank][:], in_=po, func=AF.Square),
                             R=["psF%d" % bank], W=["sq%d" % bank])
                          V(lambda e, po=po, bank=bank, fi=fg * 4 + f, th=th: e.scalar_tensor_tensor(
                              out=hid[:, fi, th * 512:(th + 1) * 512], in0=po, scalar=0.0, in1=sq[bank][:],
                              op0=ALU.is_gt, op1=ALU.mult),
                            R=["psF%d" % bank, "sq%d" % bank], W=["hid"])
                  banks8 = [(psU[:, i * 512:(i + 1) * 512], "psU%d" % i) for i in range(4)] + \
                           [(psT[:, 0:512], "psT0"), (psT[:, 512:1024], "psT1"),
                            (psF[:, 0:512], "psF0"), (psF[:, 512:1024], "psF1")]
                  for half in range(2):
                      for kg in range(8):
                          wv, wn = load_slot(lambda a, L=L, kg=kg: wdn_d[L, kg * 512 + a * 128: kg * 512 + (a + 1) * 128, :], 4)
                          for jt in range(4):
                              for nbk in range(2):
                                  bk, bn = banks8[jt * 2 + nbk]
                                  for a in range(4):
                                      k = kg * 4 + a
                                      MM(bk, hid[:, k, (half * 4 + jt) * 128:(half * 4 + jt + 1) * 128],
                                         wv[:, a, nbk * 512:(nbk + 1) * 512], start=(k == 0), stop=(k == 31),
                                         R=["hid", wn], W=[bn], sig=(a == 3))
                      for jt in range(4):
                          T = B * 8 + half * 4 + jt
                          bb = [banks8[jt * 2], banks8[jt * 2 + 1]]
                          for nbk in range(2):
                              AC(lambda e, nbk=nbk, bb=bb: e.activation(out=tmp[:, nbk * 512:(nbk + 1) * 512],
                                                                        in_=bb[nbk][0], func=AF.Square,
                                                                        accum_out=stat[:, 16 + nbk:17 + nbk]),
                                 R=[bb[nbk][1]], W=["tmp", "ss%d" % nbk])
                          V(lambda e: e.tensor_tensor(out=stat[:, 18:19], in0=stat[:, 16:17], in1=stat[:, 17:18],
                                                      op=ALU.add), R=["ss0", "ss1"], W=["ss2"])
                          rstd_from_ssq(stat[:, 18:19], stat[:, 19:20], D, epsn[:], "ss2", "ss3")
                          for nbk in range(2):
                              V(lambda e, nbk=nbk, bb=bb: e.scalar_tensor_tensor(
                                  out=tmp[:, nbk * 512:(nbk + 1) * 512],
                                  in0=bb[nbk][0], scalar=stat[:, 19:20],
                                  in1=gpb[:, nbk * 512:(nbk + 1) * 512], op0=ALU.mult, op1=ALU.mult),
                                R=[bb[nbk][1], "ss3", "gpb"], W=["tmp"])
                          G(lambda e, T=T: e.tensor_tensor(out=h[:, T, :], in0=h[:, T, :], in1=tmp[:], op=ALU.add),
                            R=["tmp", "h%d" % T], W=["h%d" % T])
              if L == 0:
                  for T in (0, 5, 15):
                      dump(h[:, T, :], D, "h%d" % T)
              P.barrier()

              A.reset()
              wg = A.bf([128, 8, D])
              wp = A.bf([128, 2, D])
              hTt = A.bf([128, 8, 128])
              ppT = A.bf([128, 2, 128])
              ptm = [A.f32([128, 256]) for _ in range(2)]
              sg = A.f32([128, D])
              tmp = A.f32([128, D])
              for c in range(8):
                  P.dma("pool", lambda e, L=L, c=c: e.dma_start(out=wg[:, c, :], in_=wg_d[L, c * 128:(c + 1) * 128, :]),
                        W=["wg"], sem="wg")
              for c in range(2):
                  P.dma("pool", lambda e, L=L, c=c: e.dma_start(out=wp[:, c, :], in_=wpl_d[L, c * 128:(c + 1) * 128, :]),
                        W=["wp"], sem="wg")
              P.group_done("wg", ["wg", "wp"])
              for T in range(NT):
                  pt = ptm[T % 2]
                  pn = "ptm%d" % (T % 2)
                  P.dma("sp", lambda e, L=L, T=T, pt=pt: e.dma_start(out=pt[:], in_=p_d[L, T * 128:(T + 1) * 128, :]),
                        W=[pn], sem=pn)
                  for c in range(8):
                      TP(psT[:, c * 128:(c + 1) * 128], h[:, T, c * 128:(c + 1) * 128], identf[:],
                         R=["h%d" % T], W=["psT%d" % (c // 4)], sig=(c % 4 == 3))
                  AC(lambda e: e.copy(out=hTt[:], in_=psT[:].rearrange("p (c t) -> p c t", c=8)),
                     R=["psT0", "psT1"], W=["hTt"])
                  for c in range(2):
                      TP(psF[:, c * 128:(c + 1) * 128], pt[:, c * 128:(c + 1) * 128], identf[:],
                         R=[pn], W=["psF0"], sig=(c == 1))
                  AC(lambda e: e.copy(out=ppT[:], in_=psF[:, 0:256].rearrange("p (c t) -> p c t", c=2)),
                     R=["psF0"], W=["ppT"])
                  for nbk in range(2):
                      for c in range(8):
                          MM(psU[:, nbk * 512:(nbk + 1) * 512], hTt[:, c, :], wg[:, c, nbk * 512:(nbk + 1) * 512],
                             start=(c == 0), stop=(c == 7), R=["hTt", "wg"], W=["psU%d" % nbk])
                      for c in range(2):
                          MM(psU[:, (2 + nbk) * 512:(3 + nbk) * 512], ppT[:, c, :], wp[:, c, nbk * 512:(nbk + 1) * 512],
                             start=(c == 0), stop=(c == 1), R=["ppT", "wp"], W=["psU%d" % (2 + nbk)])
                      AC(lambda e, nbk=nbk: e.activation(out=sg[:, nbk * 512:(nbk + 1) * 512],
                                                         in_=psU[:, nbk * 512:(nbk + 1) * 512], func=AF.Sigmoid),
                         R=["psU%d" % nbk], W=["sg"])
                      V(lambda e, nbk=nbk: e.tensor_tensor(out=tmp[:, nbk * 512:(nbk + 1) * 512],
                                                           in0=psU[:, (2 + nbk) * 512:(3 + nbk) * 512],
                                                           in1=sg[:, nbk * 512:(nbk + 1) * 512], op=ALU.mult),
                        R=["psU%d" % (2 + nbk), "sg"], W=["tmp"])
                  G(lambda e, T=T: e.tensor_tensor(out=h[:, T, :], in0=h[:, T, :], in1=tmp[:], op=ALU.add),
                    R=["tmp", "h%d" % T], W=["h%d" % T])
              P.barrier()

        except StopIteration:
            P.pe_pending = False
        for t in range(NT):
            P.dma("sp", lambda e, t=t: e.dma_start(out=out_d[t * 128:(t + 1) * 128, :], in_=h[:, t, :]),
                  R=["h%d" % t], sem="out")
        P.barrier()

        P.flush()
    return nc


_CACHE = {}


def _host_consts():
    invf = (500000.0 ** (-np.arange(0, 16, 2, dtype=np.float32) / 16.0)).astype(np.float32)
    invf = np.broadcast_to(invf[None, :], (128, 8)).copy()
    ca = np.zeros(NIT, np.float32)
    cb = np.zeros(NIT, np.float32)
    for k in range(1, NIT + 1):
        if k < NIT:
            ca[k - 1] = 2.0 ** (-k)
            cb[k - 1] = 2.0 ** (-(k + 1))
        else:
            ca[k - 1] = 2.0 ** (-NIT)
            cb[k - 1] = 2.0 ** (-NIT)
    bconst = np.broadcast_to(np.concatenate([ca, cb])[None, :], (128, 2 * NIT)).copy()
    rcc = np.zeros((128, 2, 16), np.float32)
    wins = (2, 4, 8, 16)
    for p in range(128):
        for cc in range(2):
            w = wins[cc * 2 + p // 64]
            for t in range(16):
                rcc[p, cc, t] = 1.0 / min(t + 1, w)
    return invf, bconst, rcc


def _run(inputs, nl_list, x_in, dbg=False):
    nl = len(nl_list)
    key = (nl, dbg)
    if key not in _CACHE:
        _CACHE[key] = build_program(nl, dbg)
    nc = _CACHE[key]
    f = lambda a: np.ascontiguousarray(np.asarray(a, dtype=np.float32))
    Ls = list(nl_list)
    invf, bconst, rcc = _host_consts()

    def colmajor(v, nch):
        v = f(v)[Ls]
        return np.ascontiguousarray(v.reshape(nl, nch, 128).transpose(0, 2, 1))

    smallp = np.concatenate([colmajor(inputs["conv_b"], 2), colmajor(inputs["conv_ln_g"], 2),
                             colmajor(inputs["conv_ln_b"], 2), colmajor(inputs["pool_scale"], 2)], axis=2)
    dwt = f(inputs["conv_dw"])[Ls]
    dwt = np.ascontiguousarray(dwt.reshape(nl, 31, 2, 128).transpose(0, 3, 2, 1))
    shared = {
        "invf": invf, "bconst": bconst, "rcc": rcc,
        "gpre": colmajor(inputs["norm_mix_pre"], 8), "gmlp": colmajor(inputs["norm_mlp_pre"], 8),
        "gpost": f(inputs["norm_mix_post"])[Ls].reshape(nl, 1, D),
        "gmpost": f(inputs["norm_mlp_post"])[Ls].reshape(nl, 1, D),
        "w_in": f(inputs["w_in"])[Ls], "dwt": dwt, "smallp": np.ascontiguousarray(smallp),
        "conv_pw": f(inputs["conv_pw"])[Ls], "pool_w": f(inputs["pool_w"])[Ls],
        "w_out": f(inputs["w_out"])[Ls], "w_up": f(inputs["w_up"])[Ls], "w_down": f(inputs["w_down"])[Ls],
        "ple_proj": f(inputs["ple_proj"])[Ls], "ple_gate": f(inputs["ple_gate"])[Ls],
    }
    p = f(inputs["p"])
    pos = np.asarray(inputs["positions"]).astype(np.int32)
    in_maps = []
    for b in range(8):
        m = dict(shared)
        m["x"] = np.ascontiguousarray(x_in[b])
        m["p"] = np.ascontiguousarray(p[Ls, b])
        m["post"] = np.ascontiguousarray(pos[b].reshape(NT, 128).T)
        in_maps.append(m)
    res = run_bass_kernel_spmd(nc, in_maps, core_ids=list(range(8)))
    out = np.stack([np.asarray(r["out"], dtype=np.float32) for r in res.results], axis=0)
    if dbg:
        return out, [np.asarray(r["dbg"]) for r in res.results]
    return out


def kernel(**inputs):
    x = np.asarray(inputs["x"], dtype=np.float32)
    return _run(inputs, [0, 1], x)
```

```python
import math
from contextlib import ExitStack

import numpy as np
import concourse.bass as bass
import concourse.mybir as mybir
from concourse.bass_utils import run_bass_kernel_spmd

F32 = mybir.dt.float32
BF16 = mybir.dt.bfloat16
I32 = mybir.dt.int32
ALU = mybir.AluOpType
AF = mybir.ActivationFunctionType
AX = mybir.AxisListType

S = 2048
D = 1024
NT = 16
INW = 2640
TMW = 1872
CPW = 768
DFF = 4096
NIT = 18
TOPK = 256
NEG = -30000.0
ATT_SCALE = 0.125
NORM_EPS = 1e-6
LN_EPS = 1e-5
ARENA_KB = 134


class Res:
    __slots__ = ("name", "w", "r")

    def __init__(self, name):
        self.name = name
        self.w = None
        self.r = {}


class Prog:
    ENGS = ("pe", "act", "dve", "pool", "sp")

    def __init__(self, nc, stack):
        self.nc = nc
        self.stack = stack
        self.q = {e: [] for e in self.ENGS}
        self.cnt = {e: 0 for e in self.ENGS}
        self.known = {e: {} for e in self.ENGS}
        self.sems = {}
        self.dcnt = {}
        self.resources = {}
        for e in self.ENGS:
            self.sems[e] = stack.enter_context(nc.semaphore("s_" + e))
        self.pe_pending = False

    def res(self, name):
        r = self.resources.get(name)
        if r is None:
            r = Res(name)
            self.resources[name] = r
        return r

    def _sem(self, key):
        s = self.sems.get(key)
        if s is None:
            s = self.stack.enter_context(self.nc.semaphore("s_" + key.replace(":", "_")))
            self.sems[key] = s
        return s

    def _need(self, eng, tok):
        if tok is None:
            return
        key, val = tok
        if eng == "pe" and key == "pe":
            return
        if self.known[eng].get(key, 0) >= val:
            return
        self.known[eng][key] = val
        self.q[eng].append(("w", key, val))

    def _deps(self, eng, R, W):
        for r in R:
            r = self.res(r) if isinstance(r, str) else r
            self._need(eng, r.w)
        for w in W:
            w = self.res(w) if isinstance(w, str) else w
            self._need(eng, w.w)
            for k, v in w.r.items():
                self._need(eng, (k, v))

    def _update(self, tok, R, W):
        for r in R:
            r = self.res(r) if isinstance(r, str) else r
            if r.r.get(tok[0], 0) < tok[1]:
                r.r[tok[0]] = tok[1]
        for w in W:
            w = self.res(w) if isinstance(w, str) else w
            w.w = tok
            w.r = {}

    def record(self, fn, *args):
        self.rec = [[]]
        fn(*args)
        chunks = [c for c in self.rec if c]
        self.rec = None
        return chunks

    def mark(self):
        if self.rec is not None and self.rec[-1]:
            self.rec.append([])

    def replay_merged(self, lists):
        tot = [sum(len(c) for c in l) for l in lists]
        pos = [0] * len(lists)
        done = [0] * len(lists)
        while True:
            best = None
            for i, l in enumerate(lists):
                if pos[i] < len(l):
                    fr = done[i] / max(1, tot[i])
                    if best is None or fr < best[0]:
                        best = (fr, i)
            if best is None:
                break
            i = best[1]
            for ent in lists[i][pos[i]]:
                if ent[0] == "op":
                    self.op(*ent[1:])
                else:
                    self.dma(*ent[1:])
            done[i] += len(lists[i][pos[i]])
            pos[i] += 1

    def op(self, eng, fn, R=(), W=(), sig=True):
        if getattr(self, "rec", None) is not None:
            self.rec[-1].append(("op", eng, fn, tuple(R), tuple(W), sig))
            return
        self._deps(eng, R, W)
        if sig:
            self.cnt[eng] += 1
            tok = (eng, self.cnt[eng])
            self.q[eng].append(("i", fn, [(eng, 1)]))
            if eng == "pe":
                self.pe_pending = False
        else:
            assert eng == "pe"
            tok = (eng, self.cnt[eng] + 1)
            self.q[eng].append(("i", fn, []))
            self.pe_pending = True
        self._update(tok, R, W)

    def dma(self, qeng, fn, R=(), W=(), sem=None):
        if getattr(self, "rec", None) is not None:
            self.rec[-1].append(("dma", qeng, fn, tuple(R), tuple(W), sem))
            return
        key = "d:" + sem
        for r in R:
            self._need(qeng, self.res(r).w)
        for w in W:
            w = self.res(w)
            if not (w.w is not None and w.w[0] == key):
                self._need(qeng, w.w)
            for k, v in w.r.items():
                self._need(qeng, (k, v))
        self._sem(key)
        self.dcnt[key] = self.dcnt.get(key, 0) + 16
        tok = (key, self.dcnt[key])
        self.q[qeng].append(("i", fn, [(key, 16)]))
        self._update(tok, R, W)

    def group_done(self, sem, names):
        key = "d:" + sem
        for n in names:
            self.res(n).w = (key, self.dcnt[key])

    def barrier(self):
        assert not self.pe_pending
        for e in self.ENGS:
            for o in self.ENGS:
                if o != e and self.cnt[o] > 0:
                    self._need(e, (o, self.cnt[o]))
            for key, v in self.dcnt.items():
                self._need(e, (key, v))
        for r in self.resources.values():
            r.w = None
            r.r = {}
        self.flush()

    def flush(self):
        if not any(self.q[e] for e in self.ENGS):
            return
        with self.nc.Block() as block:
            self.emit(block)
        self.q = {e: [] for e in self.ENGS}

    def emit(self, block):
        nc = self.nc
        engobj = {"pe": nc.tensor, "act": nc.scalar, "dve": nc.vector, "pool": nc.gpsimd, "sp": nc.sync}
        deco = {"pe": block.tensor, "act": block.scalar, "dve": block.vector, "pool": block.gpsimd,
                "sp": block.sync}
        for e in self.ENGS:
            lst = self.q[e]
            eo = engobj[e]

            def body(_eng, lst=lst, eo=eo):
                for ent in lst:
                    if ent[0] == "w":
                        eo.wait_ge(self.sems[ent[1]], ent[2])
                    else:
                        ins = ent[1](eo)
                        for key, amt in ent[2]:
                            ins = ins.then_inc(self.sems[key], amt)

            deco[e](body)


class Arena:
    def __init__(self, ap, nelem):
        self.ap = ap
        self.n = nelem
        self.off = 0

    def reset(self):
        self.off = 0

    def bf(self, shape):
        n = int(np.prod(shape[1:]))
        n = (n + 1) // 2 * 2
        v = self.ap[:, self.off:self.off + n]
        self.off += n
        assert self.off <= self.n, ("arena overflow", self.off, self.n)
        return self._shape(v, shape)

    def f32(self, shape):
        n = int(np.prod(shape[1:])) * 2
        v = self.ap[:, self.off:self.off + n].bitcast(F32)
        self.off += n
        assert self.off <= self.n, ("arena overflow", self.off, self.n)
        return self._shape(v, shape)

    @staticmethod
    def _shape(v, shape):
        if len(shape) == 2:
            return v
        if len(shape) == 3:
            return v.rearrange("p (a b) -> p a b", a=shape[1])
        if len(shape) == 4:
            return v.rearrange("p (a b c) -> p a b c", a=shape[1], b=shape[2])
        raise ValueError(shape)


def build_program(nl, dbg=False):
    nc = bass.Bass("TRN2", target_bir_lowering=False)

    def din(name, shape, dt=F32):
        return nc.dram_tensor(name, list(shape), dt, kind="ExternalInput").ap()

    x_d = din("x", [S, D])
    p_d = din("p", [nl, S, 256])
    pos_d = din("post", [128, NT], I32)
    invf_d = din("invf", [128, 8])
    bcn_d = din("bconst", [128, 2 * NIT])
    rcc_d = din("rcc", [128, 2, 16])
    gpre_d = din("gpre", [nl, 128, 8])
    gmlp_d = din("gmlp", [nl, 128, 8])
    gpost_d = din("gpost", [nl, 1, D])
    gmpost_d = din("gmpost", [nl, 1, D])
    win_d = din("w_in", [nl, D, INW])
    dw_d = din("dwt", [nl, 128, 2, 31])
    sm_d = din("smallp", [nl, 128, 8])
    cpw_d = din("conv_pw", [nl, 256, 256])
    plw_d = din("pool_w", [nl, 4, 64, 64])
    wout_d = din("w_out", [nl, D, D])
    wup_d = din("w_up", [nl, D, DFF])
    wdn_d = din("w_down", [nl, DFF, D])
    wpl_d = din("ple_proj", [nl, 256, D])
    wg_d = din("ple_gate", [nl, D, D])
    out_d = nc.dram_tensor("out", [S, D], F32, kind="ExternalOutput").ap()
    dbg_d = None
    if dbg:
        dbg_d = nc.dram_tensor("dbg", [128, 40000], F32, kind="ExternalOutput").ap()

    with ExitStack() as st:
        def sb(name, shape, dt=F32):
            return st.enter_context(nc.sbuf_tensor(name, list(shape), dt))

        def ps(name, shape, dt=F32):
            return st.enter_context(nc.psum_tensor(name, list(shape), dt))

        h = sb("h", [128, NT, D])
        arena_t = sb("arena", [128, ARENA_KB * 512], BF16)
        identf = sb("identf", [128, 128])
        identb = sb("identb", [128, 128], BF16)
        identb4 = sb("identb4", [128, 4, 128], BF16)
        cmask = sb("cmask", [128, 128])
        onesm = sb("onesm", [128, 128])
        cosT = sb("cosT", [128, NT, 8])
        sinT = sb("sinT", [128, NT, 8])
        posi = sb("posi", [128, NT], I32)
        posf = sb("posf", [128, NT])
        invf = sb("invf_s", [128, 8])
        ang = sb("ang", [128, NT, 8])
        ang2 = sb("ang2", [128, NT, 8])
        angi = sb("angi", [128, NT, 8], I32)
        bcn = sb("bcn", [128, 2 * NIT])
        rcc = sb("rcc_s", [128, 2, 16])
        gpre = sb("gpre_s", [128, 8])
        gmlp = sb("gmlp_s", [128, 8])
        smp = sb("smp", [128, 8])
        dwc = sb("dwc", [128, 2, 31])
        epsn = sb("epsn", [128, 1])
        epsl = sb("epsl", [128, 1])
        stat = sb("stat", [128, 64])
        rstd_all = sb("rstd_all", [128, NT])
        bis = sb("bis", [128, 4 * NIT + 16])
        dscr = sb("dscr", [128, 2048]) if dbg else None

        psT = ps("psT", [128, 1024])
        psU = ps("psU", [128, 2048])
        psF = ps("psF", [128, 1024])

        P = Prog(nc, st)
        A = Arena(arena_t, ARENA_KB * 512)

        def V(fn, R=(), W=()):
            P.op("dve", fn, R, W)

        def AC(fn, R=(), W=()):
            P.op("act", fn, R, W)

        def G(fn, R=(), W=()):
            P.op("pool", fn, R, W)

        def MM(out, lhsT, rhs, start, stop, R=(), W=(), sig=None):
            if sig is None:
                sig = stop
            P.op("pe", lambda e: e.matmul(out, lhsT=lhsT, rhs=rhs, start=start, stop=stop), R, W, sig=sig)

        def TP(out, in_, ident, R=(), W=(), sig=True):
            P.op("pe", lambda e: e.transpose(out, in_, ident), R, W, sig=sig)

        def rstd_from_ssq(ssq_ap, out_ap, n, eps_ap, rname, wname):
            AC(lambda e: e.activation(out=out_ap, in_=ssq_ap, func=AF.Sqrt, bias=eps_ap, scale=1.0 / n),
               R=[rname], W=[wname])
            V(lambda e: e.reciprocal(out=out_ap, in_=out_ap), R=[wname], W=[wname])

        for t in range(NT):
            P.dma("sp", lambda e, t=t: e.dma_start(out=h[:, t, :], in_=x_d[t * 128:(t + 1) * 128, :]),
                  W=["h%d" % t], sem="hload")
        P.group_done("hload", ["h%d" % t for t in range(NT)])
        P.dma("sp", lambda e: e.dma_start(out=posi[:], in_=pos_d), W=["posi"], sem="c0")
        P.dma("sp", lambda e: e.dma_start(out=invf[:], in_=invf_d), W=["invf"], sem="c0")
        P.dma("sp", lambda e: e.dma_start(out=bcn[:], in_=bcn_d), W=["bcn"], sem="c0")
        P.dma("sp", lambda e: e.dma_start(out=rcc[:], in_=rcc_d), W=["rcc"], sem="c0")
        P.group_done("c0", ["posi", "invf", "bcn", "rcc"])

        G(lambda e: e.memset(identf[:], 0.0), W=["identf"])
        G(lambda e: e.affine_select(out=identf[:], in_=identf[:], pattern=[[-1, 128]], compare_op=ALU.not_equal,
                                    fill=1.0, base=0, channel_multiplier=1), R=["identf"], W=["identf"])
        G(lambda e: e.tensor_copy(out=identb[:], in_=identf[:]), R=["identf"], W=["identb"])
        for c4_ in range(4):
            G(lambda e, c4_=c4_: e.tensor_copy(out=identb4[:, c4_, :], in_=identf[:]), R=["identf"], W=["identb4"])
        G(lambda e: e.memset(cmask[:], 0.0), W=["cmask"])
        G(lambda e: e.affine_select(out=cmask[:], in_=cmask[:], pattern=[[-1, 128]], compare_op=ALU.is_ge,
                                    fill=-1e30, base=0, channel_multiplier=1), R=["cmask"], W=["cmask"])
        G(lambda e: e.memset(onesm[:], 1.0 / 256.0), W=["onesm"])
        G(lambda e: e.memset(epsn[:], NORM_EPS), W=["epsn"])
        G(lambda e: e.memset(epsl[:], LN_EPS), W=["epsl"])

        V(lambda e: e.tensor_copy(out=posf[:], in_=posi[:]), R=["posi"], W=["posf"])
        V(lambda e: e.tensor_tensor(out=ang[:], in0=posf[:].unsqueeze(2).to_broadcast([128, NT, 8]),
                                    in1=invf[:].unsqueeze(1).to_broadcast([128, NT, 8]), op=ALU.mult),
          R=["posf", "invf"], W=["ang"])
        for (tab, off, nm) in ((sinT, 0.0, "sinT"), (cosT, 0.25, "cosT")):
            V(lambda e, off=off: e.tensor_scalar(out=ang2[:], in0=ang[:], scalar1=1.0 / (2 * math.pi), scalar2=off,
                                                 op0=ALU.mult, op1=ALU.add), R=["ang"], W=["ang2"])
            V(lambda e: e.tensor_copy(out=angi[:], in_=ang2[:]), R=["ang2"], W=["angi"])
            V(lambda e, tab=tab: e.tensor_copy(out=tab[:], in_=angi[:]), R=["angi"], W=[nm])
            V(lambda e, tab=tab: e.tensor_tensor(out=ang2[:], in0=ang2[:], in1=tab[:], op=ALU.subtract),
              R=["ang2", nm], W=["ang2"])
            AC(lambda e, tab=tab: e.activation(out=tab[:], in_=ang2[:], func=AF.Sin, scale=6.2831),
               R=["ang2"], W=[nm])
        P.barrier()

        dbg_slot = [0]

        def dump(ap_f32_2d, ncols, rname):
            if not dbg:
                return
            print("dump", rname, dbg_slot[0], ncols)
            if ap_f32_2d.dtype == F32:
                o = dbg_slot[0]
                P.dma("sp", lambda e: e.dma_start(out=dbg_d[:, o:o + ncols], in_=ap_f32_2d), R=[rname], sem="dbg%d" % o)
                dbg_slot[0] += ncols
                return
            for c0 in range(0, ncols, 2048):
                n = min(2048, ncols - c0)
                o = dbg_slot[0]
                V(lambda e, c0=c0, n=n: e.tensor_copy(out=dscr[:, 0:n], in_=ap_f32_2d[:, c0:c0 + n]),
                  R=[rname], W=["dscr"])
                P.dma("sp", lambda e, o=o, n=n: e.dma_start(out=dbg_d[:, o:o + n], in_=dscr[:, 0:n]),
                      R=["dscr"], sem="dbgs")
                dbg_slot[0] += n

        dump(cosT[:].rearrange("p a b -> p (a b)"), 128, "cosT")
        dump(sinT[:].rearrange("p a b -> p (a b)"), 128, "sinT")
        try:
          for L in range(nl):
              P.dma("sp", lambda e, L=L: e.dma_start(out=gpre[:], in_=gpre_d[L]), W=["gpre"], sem="c1")
              P.dma("sp", lambda e, L=L: e.dma_start(out=gmlp[:], in_=gmlp_d[L]), W=["gmlp"], sem="c1")
              P.dma("sp", lambda e, L=L: e.dma_start(out=smp[:], in_=sm_d[L]), W=["smp"], sem="c1")
              P.dma("sp", lambda e, L=L: e.dma_start(out=dwc[:], in_=dw_d[L]), W=["dwc"], sem="c1")
              P.group_done("c1", ["gpre", "gmlp", "smp", "dwc"])

              A.reset()
              mixT = A.bf([128, 8, S])
              wbig = A.bf([128, 8, TMW])
              kT = A.bf([128, S])
              kiT = A.bf([128, S])
              vext = A.bf([128, NT, 2, 65])
              aT = A.bf([128, 8, 128])
              qT = [A.bf([128, 4, 128]) for _ in range(4)]
              qiT = [A.bf([128, 8, 128]) for _ in range(2)]
              pT = [A.bf([128, 4, 128]) for _ in range(2)]
              mb = [A.bf([128, S]) for _ in range(2)]
              accs = [A.f32([128, S]) for _ in range(2)]
              hn = A.f32([128, D])
              utm = A.f32([128, TMW])
              kidup = A.f32([128, 128])
              rbuf = [A.f32([128, 512]) for _ in range(2)]
              ytm = A.f32([128, 512])
              rtA = A.f32([128, 17, 16])
              rtB = A.f32([128, 17, 16])
              qpair = A.f32([128, 512])
              wiS = [A.f32([128, 16]) for _ in range(2)]

              for c in range(8):
                  P.dma("pool", lambda e, L=L, c=c: e.dma_start(out=wbig[:, c, :],
                                                                 in_=win_d[L, c * 128:(c + 1) * 128, 0:TMW]),
                        W=["wbig"], sem="wbig")
              G(lambda e: e.memset(vext[:, :, :, 64:65], 1.0), W=["vx%d" % t for t in range(NT)])

              bTP = (psT[:, 0:512], "psT0")
              bU = [(psT[:, 512:1024], "psT1"), (psF[:, 0:512], "psF0")]
              bI = [(psU[:, 0:512], "psU0"), (psU[:, 512:1024], "psU1")]
              bS = [(psU[:, 1024:1536], "psU2"), (psU[:, 1536:2048], "psU3")]
              bPV = (psF[:, 512:1024], "psF1")

              def stageA(T):
                  hT_ = "h%d" % T
                  ts = slice(T * 128, (T + 1) * 128)
                  AC(lambda e: e.activation(out=hn[:], in_=h[:, T, :], func=AF.Square,
                                            accum_out=stat[:, 0:1]), R=[hT_], W=["hn", "st0"])
                  rstd_from_ssq(stat[:, 0:1], rstd_all[:, T:T + 1], D, epsn[:], "st0", "rstd%d" % T)
                  V(lambda e: e.tensor_scalar(out=hn[:], in0=h[:, T, :], scalar1=rstd_all[:, T:T + 1],
                                              scalar2=None, op0=ALU.mult), R=[hT_, "rstd%d" % T], W=["hn"])
                  P.mark()
                  for hf in range(2):
                      for c4 in range(4):
                          c = hf * 4 + c4
                          TP(bTP[0][:, c4 * 128:(c4 + 1) * 128], hn[:, c * 128:(c + 1) * 128], identf[:],
                             R=["hn"], W=[bTP[1]], sig=(c4 == 3))
                      V(lambda e, hf=hf: e.tensor_tensor(
                          out=aT[:, hf * 4:(hf + 1) * 4, :], in0=bTP[0].rearrange("p (c t) -> p c t", c=4),
                          in1=gpre[:, hf * 4:(hf + 1) * 4].unsqueeze(2).to_broadcast([128, 4, 128]), op=ALU.mult),
                        R=[bTP[1], "gpre"], W=["aT"])
                      P.mark()
                  cbs = [(0, 512), (512, 1024), (1024, 1536), (1536, TMW)]
                  for bi, (c0, c1) in enumerate(cbs):
                      bk, bn = bU[bi % 2]
                      for c in range(8):
                          MM(bk[:, 0:(c1 - c0)], aT[:, c, :], wbig[:, c, c0:c1],
                             start=(c == 0), stop=(c == 7), R=["aT", "wbig"], W=[bn])
                      AC(lambda e, bk=bk, c0=c0, c1=c1: e.copy(out=utm[:, c0:c1], in_=bk[:, 0:(c1 - c0)]),
                         R=[bn], W=["utm%d" % bi])
                      P.mark()
                  UT = ["utm0", "utm1", "utm2", "utm3"]
                  for (c0, nh) in ((0, 10), (768, 17)):
                      xv = utm[:, c0:c0 + nh * 64].rearrange("p (h d) -> p h d", h=nh)
                      x12 = xv[:, :, 0:16]
                      cosb = cosT[:, T:T + 1, :].to_broadcast([128, nh, 8])
                      sinb = sinT[:, T:T + 1, :].to_broadcast([128, nh, 8])
                      a_ = rtA[:, 0:nh, :]
                      b_ = rtB[:, 0:nh, :]
                      for half in range(2):
                          hs = slice(half * 8, half * 8 + 8)
                          G(lambda e, hs=hs, a_=a_, x12=x12, cosb=cosb: e.tensor_tensor(
                              out=a_[:, :, hs], in0=x12[:, :, hs], in1=cosb, op=ALU.mult), R=UT, W=["rtA"])
                          G(lambda e, hs=hs, b_=b_, x12=x12, sinb=sinb: e.tensor_tensor(
                              out=b_[:, :, hs], in0=x12[:, :, hs], in1=sinb, op=ALU.mult), R=UT, W=["rtB"])
                      G(lambda e, a_=a_, b_=b_, x12=x12: e.tensor_tensor(
                          out=x12[:, :, 0:8], in0=a_[:, :, 0:8], in1=b_[:, :, 8:16], op=ALU.subtract),
                        R=["rtA", "rtB"], W=UT)
                      G(lambda e, a_=a_, b_=b_, x12=x12: e.tensor_tensor(
                          out=x12[:, :, 8:16], in0=a_[:, :, 8:16], in1=b_[:, :, 0:8], op=ALU.add),
                        R=["rtA", "rtB"], W=UT)
                      P.mark()
                  G(lambda e: e.tensor_copy(out=kidup[:].rearrange("p (a d) -> p a d", a=2),
                                            in_=utm[:, 1792:1856].unsqueeze(1).to_broadcast([128, 2, 64])),
                    R=UT, W=["kidup"])
                  G(lambda e: e.tensor_copy(out=vext[:, T, :, 0:64],
                                            in_=utm[:, 640:768].rearrange("p (g d) -> p g d", g=2)),
                    R=UT, W=["vx%d" % T])
                  G(lambda e: e.tensor_copy(out=wiS[T % 2][:], in_=utm[:, 1856:1872]), R=UT, W=["wiS%d" % (T % 2)])
                  G(lambda e: e.tensor_copy(out=qpair[:].rearrange("p (c g d) -> p c g d", c=4, g=2),
                                            in_=utm[:, 0:512].rearrange("p (g c d) -> p c g d", g=2, c=4)),
                    R=UT, W=["qpair"])
                  P.mark()
                  qi_ = qiT[T % 2]
                  for hf in range(2):
                      for c4 in range(4):
                          c = hf * 4 + c4
                          TP(bTP[0][:, c4 * 128:(c4 + 1) * 128], utm[:, 768 + c * 128: 768 + (c + 1) * 128],
                             identf[:], R=UT, W=[bTP[1]], sig=(c4 == 3))
                      AC(lambda e, hf=hf, qi_=qi_: e.copy(out=qi_[:, hf * 4:(hf + 1) * 4, :],
                                                          in_=bTP[0].rearrange("p (c t) -> p c t", c=4)),
                         R=[bTP[1]], W=["qiT%d" % (T % 2)])
                      P.mark()
                  for c in range(4):
                      TP(bTP[0][:, c * 128:(c + 1) * 128], qpair[:, c * 128:(c + 1) * 128], identf[:], R=["qpair"],
                         W=[bTP[1]], sig=(c == 3))
                  AC(lambda e: e.copy(out=qT[T % 4][:], in_=bTP[0].rearrange("p (c t) -> p c t", c=4)),
                     R=[bTP[1]], W=["qT%d" % (T % 4)])
                  P.mark()
                  TP(bTP[0][:, 0:128], utm[:, 512:640], identf[:], R=UT, W=[bTP[1]], sig=False)
                  TP(bTP[0][:, 128:256], kidup[:], identf[:], R=["kidup"], W=[bTP[1]], sig=True)
                  AC(lambda e: e.copy(out=kT[:, ts], in_=bTP[0][:, 0:128]), R=[bTP[1]], W=["kT%d" % T])
                  AC(lambda e: e.copy(out=kiT[:, ts], in_=bTP[0][:, 128:256]), R=[bTP[1]], W=["kiT%d" % T])
                  P.mark()

              def stageI(T):
                  ns = T + 1
                  SS = ns * 128
                  nb = (SS + 511) // 512
                  qi_ = qiT[T % 2]
                  qn = "qiT%d" % (T % 2)
                  wi_ = wiS[T % 2]
                  wn = "wiS%d" % (T % 2)
                  acc = accs[T % 2]
                  accn = "acc%d" % (T % 2)
                  it = 0
                  for sbk in range(nb):
                      w = min(512, SS - sbk * 512)
                      cs = slice(sbk * 512, sbk * 512 + w)
                      kin = ["kiT%d" % j for j in range(sbk * 4, min(ns, sbk * 4 + 4))]
                      for hi in range(16):
                          c, hp = hi // 2, hi % 2
                          pr = slice(hp * 64, hp * 64 + 64)
                          bank = it % 2
                          it += 1
                          pso = bI[bank][0][:, 0:w]
                          bn = bI[bank][1]
                          MM(pso, qi_[pr, c, :], kiT[pr, cs], start=True, stop=True, R=[qn] + kin, W=[bn])
                          rb = rbuf[bank][:, 0:w]
                          AC(lambda e, rb=rb, pso=pso: e.activation(out=rb, in_=pso, func=AF.Relu),
                             R=[bn], W=["rb%d" % bank])
                          if hi == 0:
                              V(lambda e, rb=rb, cs=cs: e.tensor_scalar(out=acc[:, cs], in0=rb, scalar1=wi_[:, 0:1],
                                                                         scalar2=None, op0=ALU.mult),
                                R=["rb%d" % bank, wn], W=[accn])
                          else:
                              V(lambda e, rb=rb, cs=cs, hi=hi: e.scalar_tensor_tensor(
                                  out=acc[:, cs], in0=rb, scalar=wi_[:, hi:hi + 1], in1=acc[:, cs],
                                  op0=ALU.mult, op1=ALU.add), R=["rb%d" % bank, wn, accn], W=[accn])
                          P.mark()

              def stageBis(T):
                  ts = slice(T * 128, (T + 1) * 128)
                  ns = T + 1
                  SS = ns * 128
                  acc = accs[T % 2]
                  accn = "acc%d" % (T % 2)
                  mb_ = mb[T % 2]
                  mbn = "mb%d" % (T % 2)
                  junk = mb_
                  thr = bis[:, 0:1]
                  if T >= 2:
                      mx, mn, w0, mid, cntc, tt = (bis[:, 1:2], bis[:, 2:3], bis[:, 3:4], bis[:, 4:5], bis[:, 5:6],
                                                   bis[:, 6:7])
                      av = bis[:, 16:16 + NIT]
                      bv = bis[:, 16 + NIT:16 + 2 * NIT]
                      V(lambda e: e.tensor_reduce(out=mx, in_=acc[:, 0:SS], axis=AX.X, op=ALU.max),
                        R=[accn], W=["b_mx"])
                      V(lambda e: e.tensor_reduce(out=mn, in_=acc[:, 0:SS], axis=AX.X, op=ALU.min),
                        R=[accn], W=["b_mn"])
                      V(lambda e: e.tensor_tensor(out=w0, in0=mx, in1=mn, op=ALU.subtract),
                        R=["b_mx", "b_mn"], W=["b_w0"])
                      V(lambda e: e.tensor_scalar(out=av, in0=bcn[:, 0:NIT], scalar1=w0, scalar2=None, op0=ALU.mult),
                        R=["b_w0", "bcn"], W=["b_av"])
                      V(lambda e: e.tensor_scalar(out=bv, in0=bcn[:, NIT:2 * NIT], scalar1=w0, scalar2=None,
                                                  op0=ALU.mult), R=["b_w0", "bcn"], W=["b_bv"])
                      V(lambda e: e.scalar_tensor_tensor(out=mid, in0=w0, scalar=0.5, in1=mn, op0=ALU.mult,
                                                         op1=ALU.add), R=["b_w0", "b_mn"], W=["b_mid"])
                      P.mark()
                  V(lambda e: e.tensor_tensor(out=acc[:, ts], in0=acc[:, ts], in1=cmask[:], op=ALU.add),
                    R=[accn, "cmask"], W=[accn])
                  if T >= 2:
                      for k in range(NIT):
                          V(lambda e: e.tensor_scalar(out=junk[:, 0:SS], in0=acc[:, 0:SS], scalar1=mid,
                                                      scalar2=0.0, op0=ALU.is_ge, op1=ALU.add, accum_out=cntc),
                            R=[accn, "b_mid"], W=[mbn, "b_cnt"])
                          V(lambda e, k=k: e.tensor_scalar(out=tt, in0=cntc, scalar1=TOPK - 0.5,
                                                           scalar2=av[:, k:k + 1], op0=ALU.is_ge, op1=ALU.mult),
                            R=["b_cnt", "b_av"], W=["b_tt"])
                          dst = mid if k < NIT - 1 else thr
                          V(lambda e, k=k, dst=dst: e.scalar_tensor_tensor(out=dst, in0=tt, scalar=bv[:, k:k + 1],
                                                                           in1=mid, op0=ALU.subtract, op1=ALU.add),
                            R=["b_tt", "b_bv", "b_mid"], W=["b_mid", "b_thr"])
                          P.mark()
                  else:
                      V(lambda e: e.memset(thr, -1e29), W=["b_thr"])
                  V(lambda e: e.tensor_scalar(out=mb_[:, 0:SS], in0=acc[:, 0:SS], scalar1=thr, scalar2=NEG,
                                              op0=ALU.is_lt, op1=ALU.mult), R=[accn, "b_thr"], W=[mbn])
                  P.mark()

              def stageAtt(T):
                  ts = slice(T * 128, (T + 1) * 128)
                  ns = T + 1
                  nsb = (ns + 3) // 4
                  q_ = qT[T % 4]
                  qn = "qT%d" % (T % 4)
                  mb_ = mb[T % 2]
                  mbn = "mb%d" % (T % 2)
                  it2 = 0
                  for grp in range(2):
                      g = grp
                      pr = slice(g * 64, g * 64 + 64)
                      for si in range(ns):
                          sc = slice(si * 128, (si + 1) * 128)
                          sbank, sbn = bS[it2 % 2]
                          pb = it2 % 2
                          it2 += 1
                          MM(sbank, kT[pr, sc], q_[pr, :, :], start=True, stop=False,
                             R=["kT%d" % si, qn], W=[sbn], sig=False)
                          MM(sbank, mb_[:, sc], identb4[:], start=False, stop=True,
                             R=[mbn, "identb4"], W=[sbn], sig=True)
                          AC(lambda e, pb=pb, sbank=sbank: e.activation(
                              out=pT[pb][:], in_=sbank.rearrange("p (j t) -> p j t", j=4),
                              func=AF.Exp, scale=ATT_SCALE), R=[sbn], W=["pT%d" % pb])
                          for h4 in range(4):
                              pvo = bPV[0][:, h4 * 65: h4 * 65 + 65]
                              MM(pvo, pT[pb][:, h4, :], vext[:, si, g, :], start=(si == 0 and h4 == 0), stop=(si == ns - 1),
                                 R=["pT%d" % pb, "vx%d" % si], W=[bPV[1]], sig=(h4 == 3))
                          P.mark()
                      pv = bPV[0][:, 0:260].rearrange("p (h d) -> p h d", h=4)
                      V(lambda e, pv=pv: e.reciprocal(out=stat[:, 8:12].unsqueeze(2), in_=pv[:, :, 64:65]),
                        R=[bPV[1]], W=["rden"])
                      V(lambda e, pv=pv, grp=grp: e.tensor_tensor(
                          out=ytm[:, grp * 256:(grp + 1) * 256].rearrange("p (h d) -> p h d", h=4),
                          in0=pv[:, :, 0:64], in1=stat[:, 8:12].unsqueeze(2).to_broadcast([128, 4, 64]),
                          op=ALU.mult), R=[bPV[1], "rden"], W=["ytm"])
                      P.mark()
                  for c in range(4):
                      TP(bPV[0][:, c * 128:(c + 1) * 128], ytm[:, c * 128:(c + 1) * 128], identf[:],
                         R=["ytm"], W=[bPV[1]], sig=(c == 3))
                  AC(lambda e: e.copy(out=mixT[:, 0:4, ts], in_=bPV[0].rearrange("p (c t) -> p c t", c=4)),
                     R=[bPV[1]], W=["mixT"])
                  P.mark()

              for it_ in range(NT + 3):
                  lists = []
                  if it_ < NT:
                      lists.append(P.record(stageA, it_))
                  if 1 <= it_ <= NT:
                      lists.append(P.record(stageI, it_ - 1))
                  if 2 <= it_ <= NT + 1:
                      lists.append(P.record(stageBis, it_ - 2))
                  if it_ >= 3:
                      lists.append(P.record(stageAtt, it_ - 3))
                  P.replay_merged(lists)
              P.barrier()


              A.reset()
              mixT = A.bf([128, 8, S])
              wcp = A.bf([128, 8, CPW])
              aTb = A.bf([128, 8, 512])
              glu = [A.bf([128, 2, 542]) for _ in range(2)]
              dg = A.bf([128, 62, 128])
              zT = A.bf([128, 2, 512])
              pooled = A.bf([128, 2, 512])
              pww = A.bf([128, 2, 256])
              bd = A.bf([128, 2, 128])
              hn = A.f32([128, D])
              xp = [A.f32([128, 2, 528]) for _ in range(2)]
              yc = A.f32([128, 2, 512])
              ysq = A.f32([128, 2, 512])
              sgm = A.f32([128, 2, 512])
              mean_s = A.f32([128, 512])
              var_s = A.f32([128, 512])
              pa = A.f32([128, 2, 528])
              pb_ = A.f32([128, 2, 528])

              for c in range(8):
                  P.dma("pool", lambda e, L=L, c=c: e.dma_start(out=wcp[:, c, :],
                                                                 in_=win_d[L, c * 128:(c + 1) * 128, TMW:INW]),
                        W=["wcp"], sem="wcp")
              for c in range(2):
                  P.dma("pool", lambda e, L=L, c=c: e.dma_start(out=pww[:, c, :],
                                                                 in_=cpw_d[L, c * 128:(c + 1) * 128, :]),
                        W=["pww"], sem="wsm")
              G(lambda e: e.memset(bd[:], 0.0), W=["bd"])
              for g4 in range(4):
                  r0 = (g4 % 2) * 64
                  P.dma("pool", lambda e, L=L, g4=g4, r0=r0: e.dma_start(out=bd[r0:r0 + 64, g4 // 2, r0:r0 + 64],
                                                                         in_=plw_d[L, g4]),
                        R=[], W=["bd"], sem="wsm")
              P.group_done("wsm", ["pww", "bd"])
              for cc in range(2):
                  for j in range(31):
                      G(lambda e, cc=cc, j=j: e.tensor_scalar(out=dg[:, cc * 31 + j, :], in0=identb[:],
                                                              scalar1=dwc[:, cc, j:j + 1], scalar2=None, op0=ALU.mult),
                        R=["identb", "dwc"], W=["dg"])
              G(lambda e: e.memset(glu[1][:, :, 0:30], 0.0), W=["glu1"])
              G(lambda e: e.memset(xp[1][:], 0.0), W=["xp1"])
              G(lambda e: e.memset(glu[0][:, :, 0:30], 0.0), W=["glu0"])

              for B in range(4):
                  gb, gprev = glu[B % 2], glu[(B + 1) % 2]
                  xb, xprev = xp[B % 2], xp[(B + 1) % 2]
                  gn, gpn = "glu%d" % (B % 2), "glu%d" % ((B + 1) % 2)
                  xn, xpn = "xp%d" % (B % 2), "xp%d" % ((B + 1) % 2)
                  bs = slice(B * 512, (B + 1) * 512)
                  for jt in range(4):
                      T = B * 4 + jt
                      V(lambda e, T=T: e.tensor_scalar(out=hn[:], in0=h[:, T, :], scalar1=rstd_all[:, T:T + 1],
                                                       scalar2=None, op0=ALU.mult), R=["h%d" % T], W=["hn"])
                      for c in range(8):
                          TP(psT[:, c * 128:(c + 1) * 128], hn[:, c * 128:(c + 1) * 128], identf[:],
                             R=["hn"], W=["psT%d" % (c // 4)], sig=(c % 4 == 3))
                      V(lambda e, jt=jt: e.tensor_tensor(out=aTb[:, :, jt * 128:(jt + 1) * 128],
                                                         in0=psT[:].rearrange("p (c t) -> p c t", c=8),
                                                         in1=gpre[:].unsqueeze(2).to_broadcast([128, 8, 128]),
                                                         op=ALU.mult), R=["psT0", "psT1", "gpre"], W=["aTb"])
                  for ch in range(6):
                      bank = ch % 4
                      for c in range(8):
                          MM(psU[:, bank * 512:(bank + 1) * 512], wcp[:, c, ch * 128:(ch + 1) * 128], aTb[:, c, :],
                             start=(c == 0), stop=(c == 7), R=["wcp", "aTb"], W=["psU%d" % bank])
                      if ch in (2, 3):
                          AC(lambda e, ch=ch, bank=bank: e.activation(out=sgm[:, ch - 2, :],
                                                                      in_=psU[:, bank * 512:(bank + 1) * 512],
                                                                      func=AF.Sigmoid),
                             R=["psU%d" % bank], W=["sgm%d" % (ch - 2)])
                      if ch == 3:
                          if B > 0:
                              V(lambda e, gb=gb, gprev=gprev: e.tensor_copy(out=gb[:, :, 0:30],
                                                                            in_=gprev[:, :, 512:542]),
                                R=[gpn], W=[gn])
                          for a2 in range(2):
                              V(lambda e, a2=a2, gb=gb: e.tensor_tensor(out=gb[:, a2, 30:542],
                                                                        in0=psU[:, a2 * 512:(a2 + 1) * 512],
                                                                        in1=sgm[:, a2, :], op=ALU.mult),
                                R=["psU%d" % a2, "sgm%d" % a2], W=[gn])
                      if ch in (4, 5):
                          if ch == 4:
                              if B > 0:
                                  V(lambda e, xb=xb, xprev=xprev: e.tensor_copy(out=xb[:, :, 0:16],
                                                                                in_=xprev[:, :, 512:528]),
                                    R=[xpn], W=[xn])
                              else:
                                  V(lambda e, xb=xb: e.memset(xb[:, :, 0:16], 0.0), W=[xn])
                          AC(lambda e, ch=ch, bank=bank, xb=xb: e.copy(out=xb[:, ch - 4, 16:528],
                                                                       in_=psU[:, bank * 512:(bank + 1) * 512]),
                             R=["psU%d" % bank], W=[xn])
                  for cc in range(2):
                      bank = 2 + cc
                      for j in range(31):
                          MM(psU[:, bank * 512:(bank + 1) * 512], dg[:, cc * 31 + j, :], gb[:, cc, j:j + 512],
                             start=(j == 0), stop=(j == 30), R=["dg", gn], W=["psU%d" % bank])
                      AC(lambda e, cc=cc, bank=bank: e.activation(out=yc[:, cc, :],
                                                                  in_=psU[:, bank * 512:(bank + 1) * 512],
                                                                  func=AF.Identity, bias=smp[:, cc:cc + 1], scale=1.0),
                         R=["psU%d" % bank, "smp"], W=["yc"])
                  AC(lambda e: e.activation(out=ysq[:], in_=yc[:], func=AF.Square), R=["yc"], W=["ysq"])
                  for cc in range(2):
                      MM(psF[:, 0:512], onesm[:], yc[:, cc, :], start=(cc == 0), stop=(cc == 1),
                         R=["onesm", "yc"], W=["psF0"])
                  for cc in range(2):
                      MM(psF[:, 512:1024], onesm[:], ysq[:, cc, :], start=(cc == 0), stop=(cc == 1),
                         R=["onesm", "ysq"], W=["psF1"])
                  AC(lambda e: e.copy(out=mean_s[:], in_=psF[:, 0:512]), R=["psF0"], W=["mean_s"])
                  V(lambda e: e.tensor_tensor(out=var_s[:], in0=mean_s[:], in1=mean_s[:], op=ALU.mult),
                    R=["mean_s"], W=["var_s"])
                  V(lambda e: e.tensor_tensor(out=var_s[:], in0=psF[:, 512:1024], in1=var_s[:], op=ALU.subtract),
                    R=["psF1", "var_s"], W=["var_s"])
                  AC(lambda e: e.activation(out=var_s[:], in_=var_s[:], func=AF.Sqrt, bias=epsl[:], scale=1.0),
                     R=["var_s", "epsl"], W=["var_s"])
                  V(lambda e: e.reciprocal(out=var_s[:], in_=var_s[:]), R=["var_s"], W=["var_s"])
                  for cc in range(2):
                      V(lambda e, cc=cc: e.tensor_tensor(out=yc[:, cc, :], in0=yc[:, cc, :], in1=mean_s[:],
                                                         op=ALU.subtract), R=["yc", "mean_s"], W=["yc"])
                      V(lambda e, cc=cc: e.tensor_tensor(out=yc[:, cc, :], in0=yc[:, cc, :], in1=var_s[:],
                                                         op=ALU.mult), R=["yc", "var_s"], W=["yc"])
                      AC(lambda e, cc=cc: e.activation(out=zT[:, cc, :], in_=yc[:, cc, :], func=AF.Silu,
                                                       bias=smp[:, 4 + cc:5 + cc], scale=smp[:, 2 + cc:3 + cc]),
                         R=["yc", "smp"], W=["zT"])
                  for oc in range(2):
                      for cc in range(2):
                          MM(psF[:, oc * 512:(oc + 1) * 512], pww[:, cc, oc * 128:(oc + 1) * 128], zT[:, cc, :],
                             start=(cc == 0), stop=(cc == 1), R=["pww", "zT"], W=["psF%d" % oc])
                      AC(lambda e, oc=oc, bs=bs: e.copy(out=mixT[:, 4 + oc, bs], in_=psF[:, oc * 512:(oc + 1) * 512]),
                         R=["psF%d" % oc], W=["mixT"])
                  XR = [xn]
                  V(lambda e, xb=xb: e.tensor_tensor(out=pa[:, :, 1:528], in0=xb[:, :, 1:528], in1=xb[:, :, 0:527],
                                                     op=ALU.add), R=XR, W=["pa"])
                  V(lambda e: e.tensor_tensor(out=pb_[:, :, 3:528], in0=pa[:, :, 3:528], in1=pa[:, :, 1:526],
                                              op=ALU.add), R=["pa"], W=["pb"])
                  V(lambda e: e.tensor_tensor(out=pa[:, 1, 7:528], in0=pb_[:, 1, 7:528], in1=pb_[:, 1, 3:524],
                                              op=ALU.add), R=["pb", "pa"], W=["pa"])
                  V(lambda e: e.tensor_tensor(out=pb_[64:128, 1, 15:528], in0=pa[64:128, 1, 15:528],
                                              in1=pa[64:128, 1, 7:520], op=ALU.add), R=["pa", "pb"], W=["pb"])
                  srcs = [(pa, 0, 0, 2), (pb_, 64, 0, 4), (pa, 0, 1, 8), (pb_, 64, 1, 16)]
                  for (src, p0, cc, wdw) in srcs:
                      prt = slice(p0, p0 + 64)
                      V(lambda e, src=src, prt=prt, cc=cc, wdw=wdw, xb=xb: e.scalar_tensor_tensor(
                          out=pooled[prt, cc, :], in0=src[prt, cc, 16:528], scalar=1.0 / wdw, in1=xb[prt, cc, 16:528],
                          op0=ALU.mult, op1=ALU.subtract), R=["pa", "pb", xn], W=["pooled"])
                      if B == 0:
                          V(lambda e, src=src, prt=prt, cc=cc: e.tensor_tensor(
                              out=src[prt, cc, 16:32], in0=src[prt, cc, 16:32], in1=rcc[prt, cc, :], op=ALU.mult),
                            R=["pa", "pb", "rcc"], W=["pa", "pb"])
                          V(lambda e, src=src, prt=prt, cc=cc, xb=xb: e.tensor_tensor(
                              out=pooled[prt, cc, 0:16], in0=src[prt, cc, 16:32], in1=xb[prt, cc, 16:32],
                              op=ALU.subtract), R=["pa", "pb", xn], W=["pooled"])
                  for cc in range(2):
                      MM(psF[:, cc * 512:(cc + 1) * 512], bd[:, cc, :], pooled[:, cc, :], start=True, stop=True,
                         R=["bd", "pooled"], W=["psF%d" % cc])
                      AC(lambda e, cc=cc, bs=bs: e.activation(out=mixT[:, 6 + cc, bs],
                                                              in_=psF[:, cc * 512:(cc + 1) * 512], func=AF.Identity,
                                                              scale=smp[:, 6 + cc:7 + cc]),
                         R=["psF%d" % cc, "smp"], W=["mixT"])
              if L == 0:
                  dump(mixT[:, 4:8, :].rearrange("p a b -> p (a b)"), 4 * S, "mixT")
              P.barrier()

              def post_norm_add(T, gb_ap, gname, tmp):
                  for nbk in range(2):
                      AC(lambda e, nbk=nbk: e.activation(out=tmp[:, nbk * 512:(nbk + 1) * 512],
                                                         in_=psU[:, nbk * 512:(nbk + 1) * 512], func=AF.Square,
                                                         accum_out=stat[:, 16 + nbk:17 + nbk]),
                         R=["psU%d" % nbk], W=["tmp", "ss%d" % nbk])
                  V(lambda e: e.tensor_tensor(out=stat[:, 18:19], in0=stat[:, 16:17], in1=stat[:, 17:18], op=ALU.add),
                    R=["ss0", "ss1"], W=["ss2"])
                  rstd_from_ssq(stat[:, 18:19], stat[:, 19:20], D, epsn[:], "ss2", "ss3")
                  for nbk in range(2):
                      V(lambda e, nbk=nbk: e.scalar_tensor_tensor(out=tmp[:, nbk * 512:(nbk + 1) * 512],
                                                                  in0=psU[:, nbk * 512:(nbk + 1) * 512],
                                                                  scalar=stat[:, 19:20],
                                                                  in1=gb_ap[:, nbk * 512:(nbk + 1) * 512],
                                                                  op0=ALU.mult, op1=ALU.mult),
                        R=["psU%d" % nbk, "ss3", gname], W=["tmp"])
                  G(lambda e, T=T: e.tensor_tensor(out=h[:, T, :], in0=h[:, T, :], in1=tmp[:], op=ALU.add),
                    R=["tmp", "h%d" % T], W=["h%d" % T])

              A.reset()
              mixT = A.bf([128, 8, S])
              wout = A.bf([128, 8, D])
              gpb = A.f32([128, D])
              tmp = A.f32([128, D])
              for c in range(8):
                  P.dma("pool", lambda e, L=L, c=c: e.dma_start(out=wout[:, c, :], in_=wout_d[L, c * 128:(c + 1) * 128, :]),
                        W=["wout"], sem="wout")
              P.dma("sp", lambda e, L=L: e.dma_start(out=gpb[:], in_=gpost_d[L].to_broadcast([128, D])),
                    W=["gpb"], sem="c2")
              for T in range(NT):
                  ts = slice(T * 128, (T + 1) * 128)
                  for nbk in range(2):
                      for c in range(8):
                          MM(psU[:, nbk * 512:(nbk + 1) * 512], mixT[:, c, ts], wout[:, c, nbk * 512:(nbk + 1) * 512],
                             start=(c == 0), stop=(c == 7), R=["mixT", "wout"], W=["psU%d" % nbk])
                  post_norm_add(T, gpb, "gpb", tmp)
              if L == 0:
                  for T in (0, 5, 15):
                      dump(h[:, T, :], D, "h%d" % T)
              P.barrier()

              A.reset()
              mT = A.bf([128, 8, 1024])
              hid = A.bf([128, 32, 1024])
              NSLOT = 4
              slots = [A.bf([128, 4096]) for _ in range(NSLOT)]
              hn = A.f32([128, D])
              sq = [A.f32([128, 512]) for _ in range(2)]
              gpb = A.f32([128, D])
              tmp = A.f32([128, D])
              P.dma("sp", lambda e, L=L: e.dma_start(out=gpb[:], in_=gmpost_d[L].to_broadcast([128, D])),
                    W=["gpb"], sem="c2")
              slot_i = [0]

              def load_slot(src_fn, shape_a):
                  i = slot_i[0] % NSLOT
                  slot_i[0] += 1
                  view = slots[i].rearrange("p (a b) -> p a b", a=shape_a)
                  for a in range(shape_a):
                      P.dma("pool", lambda e, a=a, view=view: e.dma_start(out=view[:, a, :], in_=src_fn(a)),
                            W=["slot%d" % i], sem="slot%d" % i)
                  return view, "slot%d" % i

              for B in range(2):
                  for jt in range(8):
                      T = B * 8 + jt
                      AC(lambda e, T=T: e.activation(out=hn[:], in_=h[:, T, :], func=AF.Square,
                                                     accum_out=stat[:, 0:1]), R=["h%d" % T], W=["hn", "st0"])
                      rstd_from_ssq(stat[:, 0:1], stat[:, 1:2], D, epsn[:], "st0", "st1")
                      V(lambda e, T=T: e.tensor_scalar(out=hn[:], in0=h[:, T, :], scalar1=stat[:, 1:2],
                                                       scalar2=None, op0=ALU.mult), R=["h%d" % T, "st1"], W=["hn"])
                      for c in range(8):
                          TP(psT[:, c * 128:(c + 1) * 128], hn[:, c * 128:(c + 1) * 128], identf[:],
                             R=["hn"], W=["psT%d" % (c // 4)], sig=(c % 4 == 3))
                      V(lambda e, jt=jt: e.tensor_tensor(out=mT[:, :, jt * 128:(jt + 1) * 128],
                                                         in0=psT[:].rearrange("p (c t) -> p c t", c=8),
                                                         in1=gmlp[:].unsqueeze(2).to_broadcast([128, 8, 128]),
                                                         op=ALU.mult), R=["psT0", "psT1", "gmlp"], W=["mT"])
                  it3 = 0
                  for fg in range(8):
                      wv, wn = load_slot(lambda a, L=L, fg=fg: wup_d[L, a * 128:(a + 1) * 128, fg * 512:(fg + 1) * 512], 8)
                      for f in range(4):
                        for th in range(2):
                          bank = it3 % 2
                          it3 += 1
                          po = psF[:, bank * 512:(bank + 1) * 512]
                          for c in range(8):
                              MM(po, wv[:, c, f * 128:(f + 1) * 128], mT[:, c, th * 512:(th + 1) * 512],
                                 start=(c == 0), stop=(c == 7), R=[wn, "mT"], W=["psF%d" % bank])
                          AC(lambda e, po=po, bank=bank: e.activation(out=sq[bank][:], in_=po, func=AF.Square),
                             R=["psF%d" % bank], W=["sq%d" % bank])
                          V(lambda e, po=po, bank=bank, fi=fg * 4 + f, th=th: e.scalar_tensor_tensor(
                              out=hid[:, fi, th * 512:(th + 1) * 512], in0=po, scalar=0.0, in1=sq[bank][:],
                              op0=ALU.is_gt, op1=ALU.mult),
                            R=["psF%d" % bank, "sq%d" % bank], W=["hid"])
                  banks8 = [(psU[:, i * 512:(i + 1) * 512], "psU%d" % i) for i in range(4)] + \
                           [(psT[:, 0:512], "psT0"), (psT[:, 512:1024], "psT1"),
                            (psF[:, 0:512], "psF0"), (psF[:, 512:1024], "psF1")]
                  for half in range(2):
                      for kg in range(8):
                          wv, wn = load_slot(lambda a, L=L, kg=kg: wdn_d[L, kg * 512 + a * 128: kg * 512 + (a + 1) * 128, :], 4)
                          for jt in range(4):
                              for nbk in range(2):
                                  bk, bn = banks8[jt * 2 + nbk]
                                  for a in range(4):
                                      k = kg * 4 + a
                                      MM(bk, hid[:, k, (half * 4 + jt) * 128:(half * 4 + jt + 1) * 128],
                                         wv[:, a, nbk * 512:(nbk + 1) * 512], start=(k == 0), stop=(k == 31),
                                         R=["hid", wn], W=[bn], sig=(a == 3))
                      for jt in range(4):
                          T = B * 8 + half * 4 + jt
                          bb = [banks8[jt * 2], banks8[jt * 2 + 1]]
                          for nbk in range(2):
                              AC(lambda e, nbk=nbk, bb=bb: e.activation(out=tmp[:, nbk * 512:(nbk + 1) * 512],
                                                                        in_=bb[nbk][0], func=AF.Square,
                                                                        accum_out=stat[:, 16 + nbk:17 + nbk]),
                                 R=[bb[nbk][1]], W=["tmp", "ss%d" % nbk])
                          V(lambda e: e.tensor_tensor(out=stat[:, 18:19], in0=stat[:, 16:17], in1=stat[:, 17:18],
                                                      op=ALU.add), R=["ss0", "ss1"], W=["ss2"])
                          rstd_from_ssq(stat[:, 18:19], stat[:, 19:20], D, epsn[:], "ss2", "ss3")
                          for nbk in range(2):
                              V(lambda e, nbk=nbk, bb=bb: e.scalar_tensor_tensor(
                                  out=tmp[:, nbk * 512:(nbk + 1) * 512],
                                  in0=bb[nbk][0], scalar=stat[:, 19:20],
                                  in1=gpb[:, nbk * 512:(nbk + 1) * 512], op0=ALU.mult, op1=ALU.mult),
                                R=[bb[nbk][1], "ss3", "gpb"], W=["tmp"])
                          G(lambda e, T=T: e.tensor_tensor(out=h[:, T, :], in0=h[:, T, :], in1=tmp[:], op=ALU.add),
                            R=["tmp", "h%d" % T], W=["h%d" % T])
              if L == 0:
                  for T in (0, 5, 15):
                      dump(h[:, T, :], D, "h%d" % T)
              P.barrier()

              A.reset()
              wg = A.bf([128, 8, D])
              wp = A.bf([128, 2, D])
              hTt = A.bf([128, 8, 128])
              ppT = A.bf([128, 2, 128])
              ptm = [A.f32([128, 256]) for _ in range(2)]
              sg = A.f32([128, D])
              tmp = A.f32([128, D])
              for c in range(8):
                  P.dma("pool", lambda e, L=L, c=c: e.dma_start(out=wg[:, c, :], in_=wg_d[L, c * 128:(c + 1) * 128, :]),
                        W=["wg"], sem="wg")
              for c in range(2):
                  P.dma("pool", lambda e, L=L, c=c: e.dma_start(out=wp[:, c, :], in_=wpl_d[L, c * 128:(c + 1) * 128, :]),
                        W=["wp"], sem="wg")
              P.group_done("wg", ["wg", "wp"])
              for T in range(NT):
                  pt = ptm[T % 2]
                  pn = "ptm%d" % (T % 2)
                  P.dma("sp", lambda e, L=L, T=T, pt=pt: e.dma_start(out=pt[:], in_=p_d[L, T * 128:(T + 1) * 128, :]),
                        W=[pn], sem=pn)
                  for c in range(8):
                      TP(psT[:, c * 128:(c + 1) * 128], h[:, T, c * 128:(c + 1) * 128], identf[:],
                         R=["h%d" % T], W=["psT%d" % (c // 4)], sig=(c % 4 == 3))
                  AC(lambda e: e.copy(out=hTt[:], in_=psT[:].rearrange("p (c t) -> p c t", c=8)),
                     R=["psT0", "psT1"], W=["hTt"])
                  for c in range(2):
                      TP(psF[:, c * 128:(c + 1) * 128], pt[:, c * 128:(c + 1) * 128], identf[:],
                         R=[pn], W=["psF0"], sig=(c == 1))
                  AC(lambda e: e.copy(out=ppT[:], in_=psF[:, 0:256].rearrange("p (c t) -> p c t", c=2)),
                     R=["psF0"], W=["ppT"])
                  for nbk in range(2):
                      for c in range(8):
                          MM(psU[:, nbk * 512:(nbk + 1) * 512], hTt[:, c, :], wg[:, c, nbk * 512:(nbk + 1) * 512],
                             start=(c == 0), stop=(c == 7), R=["hTt", "wg"], W=["psU%d" % nbk])
                      for c in range(2):
                          MM(psU[:, (2 + nbk) * 512:(3 + nbk) * 512], ppT[:, c, :], wp[:, c, nbk * 512:(nbk + 1) * 512],
                             start=(c == 0), stop=(c == 1), R=["ppT", "wp"], W=["psU%d" % (2 + nbk)])
                      AC(lambda e, nbk=nbk: e.activation(out=sg[:, nbk * 512:(nbk + 1) * 512],
                                                         in_=psU[:, nbk * 512:(nbk + 1) * 512], func=AF.Sigmoid),
                         R=["psU%d" % nbk], W=["sg"])
                      V(lambda e, nbk=nbk: e.tensor_tensor(out=tmp[:, nbk * 512:(nbk + 1) * 512],
                                                           in0=psU[:, (2 + nbk) * 512:(3 + nbk) * 512],
                                                           in1=sg[:, nbk * 512:(nbk + 1) * 512], op=ALU.mult),
                        R=["psU%d" % (2 + nbk), "sg"], W=["tmp"])
                  G(lambda e, T=T: e.tensor_tensor(out=h[:, T, :], in0=h[:, T, :], in1=tmp[:], op=ALU.add),
                    R=["tmp", "h%d" % T], W=["h%d" % T])
              P.barrier()

        except StopIteration:
            P.pe_pending = False
        for t in range(NT):
            P.dma("sp", lambda e, t=t: e.dma_start(out=out_d[t * 128:(t + 1) * 128, :], in_=h[:, t, :]),
                  R=["h%d" % t], sem="out")
        P.barrier()

        P.flush()
    return nc


_CACHE = {}


def _host_consts():
    invf = (500000.0 ** (-np.arange(0, 16, 2, dtype=np.float32) / 16.0)).astype(np.float32)
    invf = np.broadcast_to(invf[None, :], (128, 8)).copy()
    ca = np.zeros(NIT, np.float32)
    cb = np.zeros(NIT, np.float32)
    for k in range(1, NIT + 1):
        if k < NIT:
            ca[k - 1] = 2.0 ** (-k)
            cb[k - 1] = 2.0 ** (-(k + 1))
        else:
            ca[k - 1] = 2.0 ** (-NIT)
            cb[k - 1] = 2.0 ** (-NIT)
    bconst = np.broadcast_to(np.concatenate([ca, cb])[None, :], (128, 2 * NIT)).copy()
    rcc = np.zeros((128, 2, 16), np.float32)
    wins = (2, 4, 8, 16)
    for p in range(128):
        for cc in range(2):
            w = wins[cc * 2 + p // 64]
            for t in range(16):
                rcc[p, cc, t] = 1.0 / min(t + 1, w)
    return invf, bconst, rcc


def _run(inputs, nl_list, x_in, dbg=False):
    nl = len(nl_list)
    key = (nl, dbg)
    if key not in _CACHE:
        _CACHE[key] = build_program(nl, dbg)
    nc = _CACHE[key]
    f = lambda a: np.ascontiguousarray(np.asarray(a, dtype=np.float32))
    Ls = list(nl_list)
    invf, bconst, rcc = _host_consts()

    def colmajor(v, nch):
        v = f(v)[Ls]
        return np.ascontiguousarray(v.reshape(nl, nch, 128).transpose(0, 2, 1))

    smallp = np.concatenate([colmajor(inputs["conv_b"], 2), colmajor(inputs["conv_ln_g"], 2),
                             colmajor(inputs["conv_ln_b"], 2), colmajor(inputs["pool_scale"], 2)], axis=2)
    dwt = f(inputs["conv_dw"])[Ls]
    dwt = np.ascontiguousarray(dwt.reshape(nl, 31, 2, 128).transpose(0, 3, 2, 1))
    shared = {
        "invf": invf, "bconst": bconst, "rcc": rcc,
        "gpre": colmajor(inputs["norm_mix_pre"], 8), "gmlp": colmajor(inputs["norm_mlp_pre"], 8),
        "gpost": f(inputs["norm_mix_post"])[Ls].reshape(nl, 1, D),
        "gmpost": f(inputs["norm_mlp_post"])[Ls].reshape(nl, 1, D),
        "w_in": f(inputs["w_in"])[Ls], "dwt": dwt, "smallp": np.ascontiguousarray(smallp),
        "conv_pw": f(inputs["conv_pw"])[Ls], "pool_w": f(inputs["pool_w"])[Ls],
        "w_out": f(inputs["w_out"])[Ls], "w_up": f(inputs["w_up"])[Ls], "w_down": f(inputs["w_down"])[Ls],
        "ple_proj": f(inputs["ple_proj"])[Ls], "ple_gate": f(inputs["ple_gate"])[Ls],
    }
    p = f(inputs["p"])
    pos = np.asarray(inputs["positions"]).astype(np.int32)
    in_maps = []
    for b in range(8):
        m = dict(shared)
        m["x"] = np.ascontiguousarray(x_in[b])
        m["p"] = np.ascontiguousarray(p[Ls, b])
        m["post"] = np.ascontiguousarray(pos[b].reshape(NT, 128).T)
        in_maps.append(m)
    res = run_bass_kernel_spmd(nc, in_maps, core_ids=list(range(8)))
    out = np.stack([np.asarray(r["out"], dtype=np.float32) for r in res.results], axis=0)
    if dbg:
        return out, [np.asarray(r["dbg"]) for r in res.results]
    return out


def kernel(**inputs):
    x = np.asarray(inputs["x"], dtype=np.float32)
    return _run(inputs, [0, 1], x)
```
